# Optimizing a Trainium2 kernel written in Bass

```python
import jax, jax.numpy as jnp
from jax import lax
import numpy as np

D_MODEL = 1024
BATCH = 8
SEQ = 2048
DEPTH = 2

MEM_LEN = 256
HEAD_DIM = 64
RWKV_WIDTH = D_MODEL // 2
RWKV_HEADS = RWKV_WIDTH // HEAD_DIM
NSA_WIDTH = D_MODEL - RWKV_WIDTH
NSA_HEADS = NSA_WIDTH // HEAD_DIM
NSA_KV_HEADS = 2
NSA_GROUP = NSA_HEADS // NSA_KV_HEADS
NSA_KV_WIDTH = NSA_KV_HEADS * HEAD_DIM
LORA_W = 64
LORA_A = 64
LORA_V = 32
LORA_G = 128
CMP_BLOCK = 32
CMP_STRIDE = 16
CMP_HIDDEN = 128
SEL_BLOCK = 64
SEL_TOPK = 16
WINDOW = 512
Q_BLOCK = 64
CROSS_HEADS = 4
CROSS_HEAD_DIM = D_MODEL // CROSS_HEADS
FFN_DENSE = 2816
N_EXPERTS = 8
TOP_K = 2
EXPERT_FF = 3584
MOE_BLOCK = 128
NORM_EPS = 1e-6
GN_EPS = 64e-5
NEG_INF = -1e30
FORCE_SCORE = 1e4
N_DENSE = (DEPTH + 1) // 2
N_MOE = DEPTH // 2
RWKV_COLS_FIRST = 3 * RWKV_WIDTH + LORA_W + LORA_A + LORA_G
RWKV_COLS_REST = RWKV_COLS_FIRST + LORA_V
NSA_COLS = NSA_WIDTH + 6 * NSA_KV_WIDTH + 3 * NSA_HEADS

kernel_name = "hymba_rwkv7_nsa_moe_trunk"


def rmsnorm(x, g):
    xf = x.astype(jnp.float32)
    y = xf * lax.rsqrt(jnp.mean(xf * xf, -1, keepdims=True) + NORM_EPS)
    return (y * g).astype(x.dtype)


def alibi_slopes(n):
    return jnp.asarray((2.0 ** (-8.0 * np.arange(1, n + 1) / n)).astype(np.float32))


def masked_softmax(s, mask):
    s = jnp.where(mask, s.astype(jnp.float32), NEG_INF)
    m = jnp.max(s, -1, keepdims=True)
    p = jnp.where(mask, jnp.exp(s - m), 0.0)
    return p / jnp.maximum(jnp.sum(p, -1, keepdims=True), 1e-30)


def token_shift(u, mu):
    prev = jnp.pad(u, ((0, 0), (1, 0), (0, 0)))[:, :-1]
    return u + (prev - u) * mu


def rwkv7_time_mix(u, v_first, w0, w2, a0, a2, g2, k_k, k_a, r_k, ln_w, ln_b, v0, v2):
    B, T, _ = u.shape
    H, N, W = RWKV_HEADS, HEAD_DIM, RWKV_WIDTH
    f32 = jnp.float32
    r, k, v = u[..., :W], u[..., W:2 * W], u[..., 2 * W:3 * W]
    o = 3 * W
    w_lo = u[..., o:o + LORA_W]
    o += LORA_W
    a_lo = u[..., o:o + LORA_A]
    o += LORA_A
    g_lo = u[..., o:o + LORA_G]
    o += LORA_G
    w = -jax.nn.softplus(-(w0 + jnp.tanh(w_lo) @ w2)) - 0.5
    decay = jnp.exp(-jnp.exp(w.astype(f32)))
    a = jax.nn.sigmoid(a0 + a_lo @ a2)
    if v2 is None:
        v_first = v
    else:
        v_lo = u[..., o:o + LORA_V]
        v = v + (v_first - v) * jax.nn.sigmoid(v0 + v_lo @ v2)
    g = jax.nn.sigmoid(g_lo) @ g2

    def heads(z):
        return z.astype(f32).reshape(B, T, H, N)

    kk = heads(k * k_k)
    kk = kk / jnp.maximum(jnp.sqrt(jnp.sum(kk * kk, -1, keepdims=True)), 1e-12)
    k_h = heads(k * (1 + (a - 1) * k_a))
    r_h, v_h, a_h, w_h = heads(r), heads(v), heads(a), heads(decay)
    a_vec, b_vec = -kk, kk * a_h

    def step(S, inp):
        r_t, w_t, k_t, v_t, a_t, b_t = inp
        Sa = jnp.einsum('bhij,bhj->bhi', S, a_t)
        S = S * w_t[:, :, None, :] + Sa[..., None] * b_t[:, :, None, :] + v_t[..., None] * k_t[:, :, None, :]
        return S, jnp.einsum('bhij,bhj->bhi', S, r_t)

    xs = tuple(jnp.swapaxes(z, 0, 1) for z in (r_h, w_h, k_h, v_h, a_vec, b_vec))
    _, y = lax.scan(step, jnp.zeros((B, H, N, N), f32), xs)
    y = jnp.swapaxes(y, 0, 1)
    mean = jnp.mean(y, -1, keepdims=True)
    var = jnp.mean(jnp.square(y - mean), -1, keepdims=True)
    y = ((y - mean) * lax.rsqrt(var + GN_EPS)).reshape(B, T, W) * ln_w + ln_b
    bonus = (jnp.sum(r_h * k_h * r_k, -1, keepdims=True) * v_h).reshape(B, T, W)
    y = (y + bonus) * g
    return y.astype(u.dtype), v_first


def compress_blocks(kv, pe, w1, w2):
    B, T, G, DH = kv.shape
    n_cmp = (T - CMP_BLOCK) // CMP_STRIDE + 1
    idx = np.arange(n_cmp)[:, None] * CMP_STRIDE + np.arange(CMP_BLOCK)[None, :]
    blk = kv[:, idx] + pe[:, None, :]
    blk = jnp.transpose(blk, (0, 3, 1, 2, 4)).reshape(B, G, n_cmp, CMP_BLOCK * DH)
    return jax.nn.gelu(blk @ w1) @ w2


def nsa_mix(cols, ck_pe, ck_w1, ck_w2, cv_pe, cv_w1, cv_w2):
    B, T, _ = cols.shape
    G, R, DH, KW = NSA_KV_HEADS, NSA_GROUP, HEAD_DIM, NSA_KV_WIDTH
    f32 = jnp.float32
    q = cols[..., :NSA_WIDTH].reshape(B, T, G, R, DH) * (DH ** -0.5)
    kv = cols[..., NSA_WIDTH:NSA_WIDTH + 6 * KW].reshape(B, T, 6, G, DH)
    k_c, v_c, k_s, v_s, k_w, v_w = [kv[:, :, i] for i in range(6)]
    gates = jax.nn.sigmoid(cols[..., NSA_WIDTH + 6 * KW:].astype(f32)).reshape(B, T, G, R, 3)
    slopes = alibi_slopes(NSA_HEADS).reshape(G, R)
    t_pos = jnp.arange(T)

    kc = compress_blocks(k_c, ck_pe, ck_w1, ck_w2)
    vc = compress_blocks(v_c, cv_pe, cv_w1, cv_w2)
    n_cmp = kc.shape[2]
    c_end = np.arange(n_cmp) * CMP_STRIDE + CMP_BLOCK - 1
    c_dist = (t_pos[:, None] - c_end[None, :]).astype(f32)
    s = jnp.einsum('btgrd,bgnd->bgrtn', q, kc) - slopes[:, :, None, None] * c_dist
    p_cmp = masked_softmax(s, c_dist >= 0)
    o_cmp = jnp.einsum('bgrtn,bgnd->btgrd', p_cmp, vc.astype(f32))

    n_sel = T // SEL_BLOCK
    n_top = min(SEL_TOPK, n_sel)
    c_start = np.arange(n_cmp) * CMP_STRIDE
    s_start = np.arange(n_sel) * SEL_BLOCK
    overlap = (c_start[:, None] < s_start[None, :] + SEL_BLOCK) & (c_start[:, None] + CMP_BLOCK > s_start[None, :])
    imp = jnp.einsum('bgrtn,nj->bgtj', p_cmp, jnp.asarray(overlap.astype(np.float32)))
    cur = (t_pos // SEL_BLOCK)[:, None]
    blk = jnp.arange(n_sel)[None, :]
    forced = (blk == 0) | (blk == cur) | (blk == cur - 1)
    imp = jnp.where(forced, FORCE_SCORE, imp)
    imp = jnp.where(blk > cur, -1.0, imp)
    _, sel_idx = lax.top_k(imp, n_top)

    ks_blk = jnp.swapaxes(k_s, 1, 2).reshape(B, G, n_sel, SEL_BLOCK, DH)
    vs_blk = jnp.swapaxes(v_s, 1, 2).reshape(B, G, n_sel, SEL_BLOCK, DH)
    kw_pad = jnp.pad(k_w, ((0, 0), (WINDOW, 0), (0, 0), (0, 0)))
    vw_pad = jnp.pad(v_w, ((0, 0), (WINDOW, 0), (0, 0), (0, 0)))
    b_ix = jnp.arange(B)[:, None, None, None]
    g_ix = jnp.arange(G)[None, :, None, None]
    n_chunks = T // Q_BLOCK

    def chunk(c):
        t0 = c * Q_BLOCK
        qc = lax.dynamic_slice_in_dim(q, t0, Q_BLOCK, axis=1)
        tq = t0 + jnp.arange(Q_BLOCK)
        idx = lax.dynamic_slice_in_dim(sel_idx, t0, Q_BLOCK, axis=2)
        ksel = ks_blk[b_ix, g_ix, idx].reshape(B, G, Q_BLOCK, n_top * SEL_BLOCK, DH)
        vsel = vs_blk[b_ix, g_ix, idx].reshape(B, G, Q_BLOCK, n_top * SEL_BLOCK, DH)
        spos = (idx[..., None] * SEL_BLOCK + jnp.arange(SEL_BLOCK)).reshape(B, G, Q_BLOCK, n_top * SEL_BLOCK)
        sdist = (tq[None, None, :, None] - spos).astype(f32)[:, :, None]
        s = jnp.einsum('bqgrd,bgqsd->bgrqs', qc, ksel) - slopes[None, :, :, None, None] * sdist
        p = masked_softmax(s, sdist >= 0)
        o_sel = jnp.einsum('bgrqs,bgqsd->bqgrd', p, vsel.astype(f32))
        kwc = lax.dynamic_slice_in_dim(kw_pad, t0, Q_BLOCK + WINDOW, axis=1)
        vwc = lax.dynamic_slice_in_dim(vw_pad, t0, Q_BLOCK + WINDOW, axis=1)
        wpos = t0 - WINDOW + jnp.arange(Q_BLOCK + WINDOW)
        wd = tq[:, None] - wpos[None, :]
        wmask = (wd >= 0) & (wd < WINDOW) & (wpos[None, :] >= 0)
        s = jnp.einsum('bqgrd,bsgd->bgrqs', qc, kwc) - slopes[:, :, None, None] * wd.astype(f32)
        p = masked_softmax(s, wmask)
        o_win = jnp.einsum('bgrqs,bsgd->bqgrd', p, vwc.astype(f32))
        return o_sel, o_win

    o_sel, o_win = lax.map(chunk, jnp.arange(n_chunks))
    o_sel = jnp.moveaxis(o_sel, 0, 1).reshape(B, T, G, R, DH)
    o_win = jnp.moveaxis(o_win, 0, 1).reshape(B, T, G, R, DH)
    out = gates[..., 0:1] * o_cmp + gates[..., 1:2] * o_sel + gates[..., 2:3] * o_win
    return out.reshape(B, T, NSA_WIDTH).astype(cols.dtype)


def cross_attention(h, m, wq, wkv, wo):
    B, T, D = h.shape
    M = m.shape[1]
    q = (h @ wq).reshape(B, T, CROSS_HEADS, CROSS_HEAD_DIM)
    kv = (m @ wkv).reshape(B, M, 2, CROSS_HEADS, CROSS_HEAD_DIM)
    s = jnp.einsum('bthd,bmhd->bhtm', q, kv[:, :, 0]) * (CROSS_HEAD_DIM ** -0.5)
    p = jax.nn.softmax(s.astype(jnp.float32), -1)
    o = jnp.einsum('bhtm,bmhd->bthd', p, kv[:, :, 1].astype(jnp.float32)).reshape(B, T, D)
    return o.astype(h.dtype) @ wo


def swiglu(h, wg, wu, wd):
    return (jax.nn.silu(h @ wg) * (h @ wu)) @ wd


def moe_swiglu(h, w_router, e_wg, e_wu, e_wd):
    B, T, D = h.shape
    N = B * T
    xt = h.reshape(N, D)
    logits = (xt @ w_router).astype(jnp.float32)
    top_val, top_idx = lax.top_k(logits, TOP_K)
    gate = jax.nn.softmax(top_val, -1)
    A = N * TOP_K
    flat_e = top_idx.reshape(A)
    flat_tok = jnp.repeat(jnp.arange(N), TOP_K)
    order = jnp.argsort(flat_e)
    se, stok, sgate = flat_e[order], flat_tok[order], gate.reshape(A)[order]
    counts = jnp.zeros((N_EXPERTS,), jnp.int32).at[flat_e].add(1)
    start = jnp.cumsum(counts) - counts
    padded = (counts + MOE_BLOCK - 1) // MOE_BLOCK * MOE_BLOCK
    pend = jnp.cumsum(padded)
    pstart = pend - padded
    dest = pstart[se] + jnp.arange(A) - start[se]
    n_blocks = -(-A // MOE_BLOCK) + N_EXPERTS
    P = n_blocks * MOE_BLOCK
    buf_tok = jnp.full((P,), N, jnp.int32).at[dest].set(stok)
    x_pad = jnp.concatenate([xt, jnp.zeros((1, D), xt.dtype)], 0)
    xb = x_pad[buf_tok].reshape(n_blocks, MOE_BLOCK, D)
    blk_e = jnp.minimum(jnp.searchsorted(pend, jnp.arange(n_blocks) * MOE_BLOCK, side='right'), N_EXPERTS - 1)

    def run(args):
        xblk, e = args
        return swiglu(xblk, e_wg[e], e_wu[e], e_wd[e])

    yb = lax.map(run, (xb, blk_e)).reshape(P, D)
    y = jnp.zeros((N, D), yb.dtype).at[stok].add(yb[dest] * sgate[:, None].astype(yb.dtype))
    return y.reshape(B, T, D)


def setup_inputs(seed: int = 0) -> dict:
    key = jax.random.key(seed)
    ks = iter(jax.random.split(key, 48))
    f32 = jnp.float32

    def nrm(shape, scale):
        return scale * jax.random.normal(next(ks), shape, f32)

    def unif(shape, lo, hi):
        return jax.random.uniform(next(ks), shape, f32, lo, hi)

    D, W, L, DH = D_MODEL, RWKV_WIDTH, CMP_BLOCK, HEAD_DIM
    R1 = DEPTH - 1
    return {
        "x": nrm((BATCH, SEQ, D), 1.0),
        "mem": nrm((BATCH, MEM_LEN, D), 1.0),
        "w_in_first": nrm((D, RWKV_COLS_FIRST + NSA_COLS), D ** -0.5),
        "w_in_rest": nrm((R1, D, RWKV_COLS_REST + NSA_COLS), D ** -0.5),
        "shift_mu_first": unif((RWKV_COLS_FIRST,), 0.0, 1.0),
        "shift_mu_rest": unif((R1, RWKV_COLS_REST), 0.0, 1.0),
        "mix_norm_g": 1.0 + nrm((DEPTH, D), 0.02),
        "rw_w0": unif((DEPTH, W), -6.0, 0.0),
        "rw_w2": nrm((DEPTH, LORA_W, W), 0.5 * LORA_W ** -0.5),
        "rw_a0": nrm((DEPTH, W), 0.1),
        "rw_a2": nrm((DEPTH, LORA_A, W), 0.5 * LORA_A ** -0.5),
        "rw_g2": nrm((DEPTH, LORA_G, W), LORA_G ** -0.5),
        "rw_k_k": 0.85 + nrm((DEPTH, W), 0.02),
        "rw_k_a": 1.0 + nrm((DEPTH, W), 0.02),
        "rw_r_k": nrm((DEPTH, RWKV_HEADS, DH), 0.1),
        "rw_ln_w": 1.0 + nrm((DEPTH, W), 0.02),
        "rw_ln_b": nrm((DEPTH, W), 0.02),
        "rw_v0": 0.5 + nrm((R1, W), 0.1),
        "rw_v2": nrm((R1, LORA_V, W), 0.5 * LORA_V ** -0.5),
        "cmp_k_pe": nrm((DEPTH, L, DH), 0.02),
        "cmp_k_w1": nrm((DEPTH, L * DH, CMP_HIDDEN), (L * DH) ** -0.5),
        "cmp_k_w2": nrm((DEPTH, CMP_HIDDEN, DH), CMP_HIDDEN ** -0.5),
        "cmp_v_pe": nrm((DEPTH, L, DH), 0.02),
        "cmp_v_w1": nrm((DEPTH, L * DH, CMP_HIDDEN), (L * DH) ** -0.5),
        "cmp_v_w2": nrm((DEPTH, CMP_HIDDEN, DH), CMP_HIDDEN ** -0.5),
        "w_out": nrm((DEPTH, D, D), D ** -0.5),
        "cross_norm_g": 1.0 + nrm((DEPTH, D), 0.02),
        "mem_norm_g": 1.0 + nrm((DEPTH, D), 0.02),
        "cross_wq": nrm((DEPTH, D, D), D ** -0.5),
        "cross_wkv": nrm((DEPTH, D, 2 * D), D ** -0.5),
        "cross_wo": nrm((DEPTH, D, D), D ** -0.5),
        "ffn_norm_g": 1.0 + nrm((DEPTH, D), 0.02),
        "dense_wg": nrm((N_DENSE, D, FFN_DENSE), D ** -0.5),
        "dense_wu": nrm((N_DENSE, D, FFN_DENSE), D ** -0.5),
        "dense_wd": nrm((N_DENSE, FFN_DENSE, D), FFN_DENSE ** -0.5),
        "router_w": nrm((N_MOE, D, N_EXPERTS), D ** -0.5),
        "exp_wg": nrm((N_MOE, N_EXPERTS, D, EXPERT_FF), D ** -0.5),
        "exp_wu": nrm((N_MOE, N_EXPERTS, D, EXPERT_FF), D ** -0.5),
        "exp_wd": nrm((N_MOE, N_EXPERTS, EXPERT_FF, D), EXPERT_FF ** -0.5),
        "final_norm_g": 1.0 + nrm((D,), 0.02),
    }


def reference(x, mem, w_in_first, w_in_rest, shift_mu_first, shift_mu_rest, mix_norm_g,
              rw_w0, rw_w2, rw_a0, rw_a2, rw_g2, rw_k_k, rw_k_a, rw_r_k, rw_ln_w, rw_ln_b,
              rw_v0, rw_v2, cmp_k_pe, cmp_k_w1, cmp_k_w2, cmp_v_pe, cmp_v_w1, cmp_v_w2,
              w_out, cross_norm_g, mem_norm_g, cross_wq, cross_wkv, cross_wo,
              ffn_norm_g, dense_wg, dense_wu, dense_wd, router_w, exp_wg, exp_wu, exp_wd,
              final_norm_g):
    v_first = None
    for layer in range(DEPTH):
        h = rmsnorm(x, mix_norm_g[layer])
        if layer == 0:
            w_in, mu, v0, v2 = w_in_first, shift_mu_first, None, None
        else:
            w_in, mu = w_in_rest[layer - 1], shift_mu_rest[layer - 1]
            v0, v2 = rw_v0[layer - 1], rw_v2[layer - 1]
        proj = h @ w_in
        n_rw = w_in.shape[1] - NSA_COLS
        rw_cols = token_shift(proj[..., :n_rw], mu)
        y_rw, v_first = rwkv7_time_mix(rw_cols, v_first, rw_w0[layer], rw_w2[layer], rw_a0[layer],
                                       rw_a2[layer], rw_g2[layer], rw_k_k[layer], rw_k_a[layer],
                                       rw_r_k[layer], rw_ln_w[layer], rw_ln_b[layer], v0, v2)
        y_nsa = nsa_mix(proj[..., n_rw:], cmp_k_pe[layer], cmp_k_w1[layer], cmp_k_w2[layer],
                        cmp_v_pe[layer], cmp_v_w1[layer], cmp_v_w2[layer])
        x = x + jnp.concatenate([y_rw, y_nsa], -1) @ w_out[layer]
        h = rmsnorm(x, cross_norm_g[layer])
        m = rmsnorm(mem, mem_norm_g[layer])
        x = x + cross_attention(h, m, cross_wq[layer], cross_wkv[layer], cross_wo[layer])
        h = rmsnorm(x, ffn_norm_g[layer])
        i = layer // 2
        if layer % 2 == 0:
            x = x + swiglu(h, dense_wg[i], dense_wu[i], dense_wd[i])
        else:
            x = x + moe_swiglu(h, router_w[i], exp_wg[i], exp_wu[i], exp_wd[i])
    return rmsnorm(x, final_norm_g)
```

```python
import bisect
import numpy as np
from contextlib import ExitStack
import concourse.bass as bass
import concourse.mybir as mybir
from concourse.bass_utils import run_bass_kernel_spmd

F32 = mybir.dt.float32
BF16 = mybir.dt.bfloat16
I32 = mybir.dt.int32
U32 = mybir.dt.uint32
AF = mybir.ActivationFunctionType
ALU = mybir.AluOpType
AX = mybir.AxisListType
DSIZE = {F32: 4, BF16: 2, I32: 4, U32: 4}

ENG = ("pe", "dve", "act", "pool", "sp")
RING = {"sp": 16, "act": 4, "pool": 8}
ARENA_WORDS = 49152


class Buf:
    def __init__(self, ap, key, w0, w1):
        self.ap, self.key, self.w0, self.w1 = ap, key, w0, w1
        self.shape = tuple(ap.shape)

    def __getitem__(self, idx):
        return self.ap[idx]


class Prog:
    def __init__(self, same_engine_sync=True):
        self.nc = bass.Bass("TRN2", target_bir_lowering=False)
        self.es = ExitStack()
        self.ops = {e: [] for e in ENG}
        self.cnt = {e: 0 for e in ENG}
        self.sems = {}
        self.res = {}
        self.waited = {e: {} for e in ENG}
        self.pending = {e: [] for e in ENG}
        self.ring_pos = {q: 0 for q in RING}
        self.ring_val = {q: [0] * RING[q] for q in RING}
        self.same_engine_sync = same_engine_sync
        for e in ("pe", "dve", "act", "pool"):
            self.sems[e] = self.es.enter_context(self.nc.semaphore("s_" + e))
        for q in RING:
            for i in range(RING[q]):
                self.sems[(q, i)] = self.es.enter_context(self.nc.semaphore("r_%s_%d" % (q, i)))
        self.arena = self.es.enter_context(self.nc.sbuf_tensor("arena", [128, ARENA_WORDS], F32))
        self.top = 0
        self.allocs = []
        self.nalloc = 0
        self.banks = [self.es.enter_context(self.nc.psum_tensor("ps%d" % i, [128, 512], F32)) for i in range(8)]
        self.bank_i = 0

    def alloc(self, name, shape, dtype=F32, split=False):
        p = shape[0]
        n = int(np.prod(shape[1:]))
        words = (n * DSIZE[dtype] + 3) // 4
        words = (words + 7) // 8 * 8
        w0 = self.top
        self.top += words
        assert self.top <= ARENA_WORDS, "SBUF arena overflow at %s: %d" % (name, self.top)
        ap = self.arena[0:p, w0:w0 + words]
        if dtype != F32:
            ap = ap.bitcast(dtype)
        ap = ap[:, 0:n]
        if len(shape) > 2:
            names = " ".join("d%d" % i for i in range(len(shape) - 1))
            kw = {"d%d" % i: shape[i + 1] for i in range(len(shape) - 1)}
            ap = ap.rearrange("p (%s) -> p %s" % (names, names), **kw)
        self.nalloc += 1
        key = "%s#%d" % (name, self.nalloc)
        if split:
            key = key + "/S"
        self.allocs.append((w0, w0 + words, key))
        return Buf(ap, key, w0, w0 + words)

    def mark(self):
        return (self.top, len(self.allocs))

    def release(self, m):
        self.barrier()
        self.top = m[0]
        del self.allocs[m[1]:]

    def psum(self):
        b = self.banks[self.bank_i]
        self.bank_i = (self.bank_i + 1) % 8
        return b

    def dram(self, name, shape, dtype=F32, kind="Internal"):
        return self.nc.dram_tensor(name, list(shape), dtype, kind=kind)

    def key_of(self, ap):
        if isinstance(ap, Buf):
            if ap.key.endswith("/S"):
                return [ap.key + "lo", ap.key + "hi"]
            return ap.key
        if isinstance(ap, (str, tuple)):
            return ap
        t = ap.tensor
        if t.name == "arena":
            row = t.shape[1]
            col = ap.offset % row
            w = col * DSIZE[ap.dtype] // 4
            i = bisect.bisect_right(self.allocs, (w, 1 << 60, "")) - 1
            a = self.allocs[i]
            assert a[0] <= w < a[1], "AP outside allocations"
            if a[2].endswith("/S"):
                p0 = ap.offset // row
                p1 = p0 + ap.shape[0]
                ks = []
                if p0 < 64:
                    ks.append(a[2] + "lo")
                if p1 > 64:
                    ks.append(a[2] + "hi")
                return ks
            return a[2]
        return t.name

    def keys(self, *aps):
        out = []
        for a in aps:
            if a is None or isinstance(a, (int, float)):
                continue
            for x in (a if isinstance(a, list) else [a]):
                k = self.key_of(x)
                if isinstance(k, list):
                    out.extend(k)
                else:
                    out.append(k)
        return out

    def _deps(self, eng, reads, writes):
        need = {}

        def add(tok, hazard):
            if tok is None:
                return
            s, v, e = tok
            if e == eng:
                if eng == "pe" or not self.same_engine_sync:
                    return
            if need.get(s, (0,))[0] < v:
                need[s] = (v, e)

        for k in reads:
            st = self.res.get(k)
            if st is not None:
                add(st["w"], "raw")
        for k in writes:
            st = self.res.get(k)
            if st is not None:
                add(st["w"], "waw")
                for s, (v, e) in st["r"].items():
                    add((s, v, e), "war")
        out = list(self.pending[eng])
        self.pending[eng] = []
        wd = self.waited[eng]
        for s, v in out:
            if wd.get(s, 0) < v:
                wd[s] = v
        for s, (v, e) in need.items():
            if wd.get(s, 0) < v:
                wd[s] = v
                out.append((s, v))
        return out

    def _mark(self, tok, reads, writes):
        s, v, e = tok
        for k in reads:
            st = self.res.setdefault(k, {"w": None, "r": {}})
            st["r"][s] = (v, e)
        for k in writes:
            self.res[k] = {"w": tok, "r": {}}

    def op(self, eng, fn, reads=(), writes=()):
        reads = self.keys(*reads)
        writes = self.keys(*writes)
        waits = self._deps(eng, reads, writes)
        self.cnt[eng] += 1
        tok = (eng, self.cnt[eng], eng)
        self.ops[eng].append((waits, fn, (eng, 1)))
        self._mark(tok, reads, writes)

    def dma(self, q, out, in_, rk=None, wk=None, **kw):
        reads = self.keys(in_) if rk is None else self.keys(*rk)
        writes = self.keys(out) if wk is None else self.keys(*wk)
        pos = self.ring_pos[q]
        self.ring_pos[q] = (pos + 1) % RING[q]
        waits = self._deps(q, reads, writes)
        prev = self.ring_val[q][pos]
        s = (q, pos)
        if prev > 0 and self.waited[q].get(s, 0) < prev:
            self.waited[q][s] = prev
            waits.append((s, prev))
        val = prev + 16
        self.ring_val[q][pos] = val
        self.ops[q].append((waits, lambda e: e.dma_start(out=out, in_=in_, **kw), (s, 16)))
        self._mark((s, val, "dma"), reads, writes)

    def _all_tokens(self):
        fin = []
        for e in ("pe", "dve", "act", "pool"):
            if self.cnt[e] > 0:
                fin.append((e, self.cnt[e]))
        for q in RING:
            for i in range(RING[q]):
                if self.ring_val[q][i] > 0:
                    fin.append(((q, i), self.ring_val[q][i]))
        return fin

    def barrier(self):
        fin = self._all_tokens()
        for e in ENG:
            self.pending[e] = [(s, v) for (s, v) in fin if s != e or True]
        self.res = {}

    def mm(self, out, lhsT, rhs, start=True, stop=True, rk=None, wk=None):
        self.op("pe", lambda e: e.matmul(out, lhsT=lhsT, rhs=rhs, start=start, stop=stop),
                reads=[lhsT, rhs] if rk is None else rk, writes=[out] if wk is None else wk)

    def tr(self, out, in_, ident):
        self.op("pe", lambda e: e.transpose(out, in_, ident), reads=[in_, ident], writes=[out])

    def act(self, out, in_, func, bias=None, scale=None, accum_out=None, rk=None, wk=None):
        kw = {}
        if bias is not None:
            kw["bias"] = bias
        if scale is not None:
            kw["scale"] = scale
        if accum_out is not None:
            kw["accum_out"] = accum_out
        self.op("act", lambda e: e.activation(out=out, in_=in_, func=func, **kw),
                reads=[in_, bias, scale] if rk is None else rk,
                writes=[out, accum_out] if wk is None else wk)

    def tt(self, out, in0, in1, op, eng="dve", rk=None, wk=None):
        self.op(eng, lambda e: e.tensor_tensor(out=out, in0=in0, in1=in1, op=op),
                reads=[in0, in1] if rk is None else rk, writes=[out] if wk is None else wk)

    def ts(self, out, in0, s1, s2=None, op0=ALU.mult, op1=None, eng="dve", accum_out=None, rk=None, wk=None):
        kw = {}
        if op1 is not None:
            kw["op1"] = op1
        if accum_out is not None:
            kw["accum_out"] = accum_out
        self.op(eng, lambda e: e.tensor_scalar(out=out, in0=in0, scalar1=s1, scalar2=s2, op0=op0, **kw),
                reads=[in0, s1, s2] if rk is None else rk, writes=[out, accum_out] if wk is None else wk)

    def stt(self, out, in0, scalar, in1, op0, op1, rk=None, wk=None):
        self.op("dve", lambda e: e.scalar_tensor_tensor(out=out, in0=in0, scalar=scalar, in1=in1, op0=op0, op1=op1),
                reads=[in0, scalar, in1] if rk is None else rk, writes=[out] if wk is None else wk)

    def copy(self, out, in_, eng="dve", rk=None, wk=None):
        if eng == "act":
            fn = lambda e: e.copy(out=out, in_=in_)
        else:
            fn = lambda e: e.tensor_copy(out=out, in_=in_)
        self.op(eng, fn, reads=[in_] if rk is None else rk, writes=[out] if wk is None else wk)

    def memset(self, ap, val, eng="dve"):
        self.op(eng, lambda e: e.memset(ap, val), reads=[], writes=[ap])

    def recip(self, out, in_):
        self.op("dve", lambda e: e.reciprocal(out=out, in_=in_), reads=[in_], writes=[out])

    def finish(self):
        nc = self.nc
        fin = self._all_tokens()
        engmap = {"pe": "tensor", "dve": "vector", "act": "scalar", "pool": "gpsimd", "sp": "sync"}
        with nc.Block() as block:
            for e in ENG:
                ops = self.ops[e]
                last = fin if e == "sp" else []
                if not ops and not last:
                    continue

                def body(eng, ops=ops, last=last):
                    for waits, fn, (s, n) in ops:
                        for ws, wv in waits:
                            eng.wait_ge(self.sems[ws], wv)
                        fn(eng).then_inc(self.sems[s], n)
                    for ws, wv in last:
                        eng.wait_ge(self.sems[ws], wv)

                getattr(block, engmap[e])(body)
        self.es.close()
        return nc


import numpy as np, time, sys

T = 2048
D = 1024
EPS = 1e-6
GN_EPS = 64e-5
NEG = -30000.0
SLOPES = [2.0 ** (-(h + 1)) for h in range(8)]


def host_consts():
    c = {}
    c["ones_f"] = np.ones((128, 128), np.float32)
    blk = np.zeros((128, 128), np.float32)
    blk[:64, :64] = 1
    blk[64:, 64:] = 1
    c["blk_f"] = blk
    c["ident_f"] = np.eye(128, dtype=np.float32)
    t = np.arange(T)
    c["rmask"] = np.broadcast_to((t % 64 != 0).astype(np.float32)[None, :], (128, T)).copy()
    r = np.arange(64)[:, None]
    col = np.arange(64)[None, :]
    lo = (col < r).astype(np.float32)
    up = (r < col).astype(np.float32)
    upi = (r <= col).astype(np.float32)
    m2 = np.zeros((128, 4, 2, 64), np.float32)
    m2[:, :, 0, :] = np.concatenate([lo, lo], 0)[:, None, :]
    m2[:, :, 1, :] = np.concatenate([up, up], 0)[:, None, :]
    c["rwm2"] = m2.reshape(128, 512)
    m3 = np.zeros((128, 2, 3, 64), np.float32)
    m3[:, :, 0, :] = np.concatenate([up, up], 0)[:, None, :]
    m3[:, :, 1, :] = np.concatenate([upi, upi], 0)[:, None, :]
    m3[:, :, 2, :] = np.concatenate([upi, upi], 0)[:, None, :]
    c["rwm3"] = m3.reshape(128, 384)
    idb = np.zeros((128, 4, 64), np.float32)
    idb[:] = np.concatenate([np.eye(64), np.eye(64)], 0).astype(np.float32)[:, None, :]
    c["identbc"] = idb.reshape(128, 256)
    aq = np.zeros((8, 2, T), np.float32)
    tl = t % 512
    for h in range(8):
        aq[h, 0] = -SLOPES[h] * (tl % 256)
        aq[h, 1] = -SLOPES[h] * (tl - tl % 256)
    c["alibi_q"] = aq.reshape(16, T)
    sl = np.arange(128)[:, None]
    tl5 = np.arange(512)[None, :]
    cneg = np.zeros((128, 4, 512), np.float32)
    wneg = np.zeros((128, 4, 512), np.float32)
    for o in range(4):
        cneg[:, o, :] = np.where(tl5 >= 128 * o + sl, 0.0, NEG)
        wneg[:, o, :] = np.where(tl5 < sl + 128 * o, 0.0, NEG)
    c["cneg"] = cneg.reshape(128, 2048)
    c["wneg"] = wneg.reshape(128, 2048)
    n = np.arange(128)[:, None]
    c["cmpneg"] = np.where(t[None, :] >= 16 * n + 31, 0.0, NEG).astype(np.float32)
    bsw = np.zeros((128, 8, 16), np.float32)
    for h in range(8):
        for idx in range(16):
            bsw[:, h, idx] = SLOPES[h] * (np.arange(128) - 128 * (idx - 3))
    c["bias_sw"] = bsw.reshape(128, 128)
    bc = np.zeros((128, 8, 4), np.float32)
    for h in range(8):
        for tt in range(4):
            bc[:, h, tt] = SLOPES[h] * (16 * np.arange(128) + 31 - 512 * tt)
    c["bias_cmp"] = bc.reshape(128, 32)
    j = np.arange(32)[None, :]
    ov = ((16 * n < 64 * j + 64) & (16 * n + 32 > 64 * j)).astype(np.float32)
    c["ov"] = ov
    ta = np.zeros((128, 8, 32), np.float32)
    tb = np.zeros((128, 8, 32), np.float32)
    for mi, m in enumerate(range(8, 16)):
        tt_ = 128 * m + np.arange(128)
        cur = (tt_ // 64)[:, None]
        forced = (j == 0) | (j == cur) | (j == cur - 1)
        fut = j > cur
        ta[:, mi, :] = 1.0 - forced - fut
        tb[:, mi, :] = 1e4 * forced - 1.0 * fut
    c["topA"] = ta.reshape(128, 256)
    c["topB"] = tb.reshape(128, 256)
    ee = np.zeros((32, 16, 128), np.float32)
    for kt in range(16):
        for s in range(128):
            ee[2 * kt + s // 64, kt, s] = 1.0
    c["eexp"] = ee.reshape(32, 2048)
    oh = np.zeros((8, 8, 128), np.float32)
    for e in range(8):
        oh[e, e, :] = 1.0
    c["onehot8"] = oh.reshape(8, 1024)
    return c


class Ctx:
    stop = 99


class StopBuild(Exception):
    pass


def chk(X, n):
    if X.stop == n:
        raise StopBuild()


def col_load(P, dst, vec, q="sp"):
    P.dma(q, dst, vec.rearrange("(c p o) -> p c o", p=128, o=1), allow_slow_non_contiguous=True)


def load_consts(P, cin):
    C = {}
    for k in ("ones_f", "blk_f", "ident_f"):
        C[k] = P.alloc(k, [128, 128], F32)
        P.dma("sp", C[k][:], cin[k])
    C["ident_b"] = P.alloc("ident_b", [128, 128], BF16)
    P.copy(C["ident_b"][:], C["ident_f"][:])
    C["ones_b"] = P.alloc("ones_b", [128, 128], BF16)
    P.copy(C["ones_b"][:], C["ones_f"][:])
    C["eps"] = P.alloc("eps", [128, 1], F32)
    P.memset(C["eps"][:], EPS)
    C["gneps"] = P.alloc("gneps", [128, 1], F32)
    P.memset(C["gneps"][:], GN_EPS)
    C["one"] = P.alloc("one", [128, 1], F32)
    P.memset(C["one"][:], 1.0)
    C["mhalf"] = P.alloc("mhalf", [128, 1], F32)
    P.memset(C["mhalf"][:], -0.5)
    return C


def rmsnorm_T(P, C, src, g, hT, Tn, h32cb=None):
    m = P.mark()
    W = min(512, Tn)
    gcol = P.alloc("gcol", [128, 8, 1], F32)
    col_load(P, gcol[:], g)
    xts = [P.alloc("xt%d" % i, [128, 8, W], F32) for i in range(2)]
    sqs = [P.alloc("sq%d" % i, [128, 8, W], F32) for i in range(2)]
    rs = [P.alloc("rs%d" % i, [128, W], F32) for i in range(2)]
    for tt in range(Tn // W):
        xt = xts[tt % 2]; sq = sqs[tt % 2]; r = rs[tt % 2]
        P.dma("sp", xt[:], src[:, tt * W:(tt + 1) * W].rearrange("(c p) t -> p c t", p=128))
        P.act(sq[:], xt[:], AF.Square)
        ps = P.psum()
        for dc in range(8):
            P.mm(ps[:, :W], C["ones_f"][:], sq[:, dc, :], start=(dc == 0), stop=(dc == 7))
        P.act(r[:], ps[:, :W], AF.Sqrt, bias=C["eps"][:], scale=1.0 / D)
        P.recip(r[:], r[:])
        for dc in range(8):
            if h32cb is None:
                P.stt(hT[:, dc, tt * W:(tt + 1) * W], xt[:, dc, :], gcol[:, dc, :], r[:], ALU.mult, ALU.mult)
            else:
                h32cb(tt, dc, xt, gcol, r)
    P.release(m)


def proj_psum(P, X, wsb, n, tt):
    ps = P.psum()
    for dc in range(8):
        P.mm(ps[:n, :], wsb[:, dc, :n], X.hT[:, dc, tt * 512:(tt + 1) * 512], start=(dc == 0), stop=(dc == 7))
    return ps


def load_w(P, wsb, w, c0, n, kc=8):
    P.dma("pool", wsb[:, :, :n], w[:, c0:c0 + n].rearrange("(c p) n -> p c n", p=128))


def proj_shift(P, X, c0, n, dst, raw, wsbs, mucols):
    X.psi = getattr(X, "psi", 0) + 1
    wsb = wsbs[X.psi % len(wsbs)]
    mucol = mucols[X.psi % len(mucols)]
    load_w(P, wsb, X.w_in, c0, n)
    P.dma("sp", mucol[:n, :], X.mu[c0:c0 + n].rearrange("(p o) -> p o", o=1), allow_slow_non_contiguous=True)
    for tt in range(4):
        ps = proj_psum(P, X, wsb, n, tt)
        P.copy(raw[:n, 1 + tt * 512:1 + (tt + 1) * 512], ps[:n, :], eng="act")
    P.tt(dst[:n, :], raw[:n, 0:T], raw[:n, 1:T + 1], ALU.subtract)
    P.stt(dst[:n, :], dst[:n, :], mucol[:n, :], raw[:n, 1:T + 1], ALU.mult, ALU.add)


def rwkv_phase(P, C, X, L):
    cin = X.cin
    m0 = P.mark()
    rwm2 = P.alloc("rwm2", [128, 512], BF16)
    P.dma("pool", rwm2[:], cin["rwm2"])
    rwm3 = P.alloc("rwm3", [128, 384], BF16)
    P.dma("pool", rwm3[:], cin["rwm3"])
    identbc = P.alloc("identbc", [128, 256], BF16)
    P.dma("pool", identbc[:], cin["identbc"])
    rmask = P.alloc("rmask", [128, T], F32)
    P.dma("sp", rmask[:], cin["rmask"])
    cols = {}
    for nm in ("rw_w0", "rw_a0", "rw_k_k", "rw_k_a", "rw_ln_w", "rw_ln_b"):
        cols[nm] = P.alloc(nm, [128, 4, 1], F32)
        col_load(P, cols[nm][:], X.inp[nm][L])
    cols["rk"] = P.alloc("rk", [128, 4, 1], F32)
    col_load(P, cols["rk"][:], X.inp["rw_r_k"][L].rearrange("h d -> (h d)"))
    if L > 0:
        cols["v0"] = P.alloc("v0", [128, 4, 1], F32)
        col_load(P, cols["v0"][:], X.inp["rw_v0"][L - 1])
    negw0 = P.alloc("negw0", [128, 4, 1], F32)
    P.ts(negw0[:], cols["rw_w0"][:], -1.0)
    omka = P.alloc("omka", [128, 4, 1], F32)
    P.ts(omka[:], cols["rw_k_a"][:], -1.0, 1.0, ALU.mult, ALU.add)
    wa2 = P.alloc("wa2", [128, 512], BF16)
    P.dma("pool", wa2[0:64, :], X.inp["rw_w2"][L])
    P.dma("pool", wa2[64:128, :], X.inp["rw_a2"][L])
    g2 = P.alloc("g2", [128, 512], BF16)
    P.dma("pool", g2[:], X.inp["rw_g2"][L])
    if L > 0:
        v2 = P.alloc("v2", [32, 512], BF16)
        P.dma("pool", v2[:], X.inp["rw_v2"][L - 1])
    lw_a = P.alloc("lw_a", [128, T], BF16)
    sglo = P.alloc("sglo", [128, T], BF16)
    vlo = P.alloc("vlo", [32, T], BF16) if L > 0 else None
    wsb = [P.alloc("wsb%d" % i, [128, 8, 128], BF16) for i in range(3)]
    mucol = [P.alloc("mucol%d" % i, [128, 1], F32) for i in range(3)]
    raw = P.alloc("raw", [128, T + 1], F32)
    P.memset(raw[:, 0:1], 0.0)
    m1 = P.mark()
    tmp = P.alloc("tmp", [128, T], F32)
    proj_shift(P, X, 1536, 128, tmp, raw, wsb, mucol)
    P.act(lw_a[0:64, :], tmp[0:64, :], AF.Tanh)
    P.copy(lw_a[64:128, :], tmp[64:128, :], eng="act")
    proj_shift(P, X, 1664, 128, tmp, raw, wsb, mucol)
    P.act(sglo[:], tmp[:], AF.Sigmoid)
    if L > 0:
        proj_shift(P, X, 1792, 32, tmp, raw, wsb, mucol)
        P.copy(vlo[:], tmp[0:32, :], eng="act")
    P.release(m1)
    chk(X, 1)

    for ct in range(4):
        mc = P.mark()
        cs = slice(ct * 128, (ct + 1) * 128)
        gT = P.alloc("gT", [128, T], F32)
        bonus = P.alloc("bonus", [128, T], F32)
        mA = P.mark()
        Rt = P.alloc("Rt", [128, T], BF16)
        Kt = P.alloc("Kt", [128, T], BF16)
        Bt = P.alloc("Bt", [128, T], BF16)
        At = P.alloc("At", [128, T], BF16)
        Kh = P.alloc("Kh", [128, T], BF16)
        Bh = P.alloc("Bh", [128, T], BF16)
        Vb = P.alloc("Vb", [128, T], BF16)
        gam = P.alloc("gam", [128, 32], F32)
        mp = P.mark()
        r = P.alloc("r", [128, T], F32)
        k = P.alloc("k", [128, T], F32)
        v = P.alloc("v", [128, T], F32)
        a = P.alloc("a", [128, T], F32)
        e2 = P.alloc("e2", [128, T], F32)
        csn = P.alloc("csn", [128, T], F32)
        kkn = P.alloc("kkn", [128, T], F32)
        b = P.alloc("b", [128, T], F32)
        t1 = P.alloc("t1", [128, T], F32)
        proj_shift(P, X, ct * 128, 128, r, raw, wsb, mucol)
        proj_shift(P, X, 512 + ct * 128, 128, k, raw, wsb, mucol)
        proj_shift(P, X, 1024 + ct * 128, 128, v, raw, wsb, mucol)
        for tt in range(4):
            ts_ = slice(tt * 512, (tt + 1) * 512)
            ps = P.psum()
            P.mm(ps[:, :], wa2[0:64, cs], lw_a[0:64, ts_])
            P.act(t1[:, ts_], ps[:, :], AF.Exp, bias=negw0[:, ct, :], scale=-1.0)
            ps = P.psum()
            P.mm(ps[:, :], wa2[64:128, cs], lw_a[64:128, ts_])
            P.act(a[:, ts_], ps[:, :], AF.Sigmoid, bias=cols["rw_a0"][:, ct, :])
            ps = P.psum()
            P.mm(ps[:, :], g2[:, cs], sglo[:, ts_])
            P.copy(gT[:, ts_], ps[:, :], eng="act")
        P.act(t1[:], t1[:], AF.Ln, bias=C["one"][:])
        P.act(e2[:], t1[:], AF.Exp, bias=C["mhalf"][:], scale=-1.0)
        if L > 0:
            vf = b
            P.dma("sp", vf[:], X.vfirst[cs, :])
            for tt in range(4):
                ts_ = slice(tt * 512, (tt + 1) * 512)
                ps = P.psum()
                P.mm(ps[:, :], v2[0:32, cs], vlo[0:32, ts_])
                P.act(t1[:, ts_], ps[:, :], AF.Sigmoid, bias=cols["v0"][:, ct, :])
            P.tt(vf[:], vf[:], v[:], ALU.subtract)
            P.tt(vf[:], vf[:], t1[:], ALU.mult)
            P.tt(v[:], v[:], vf[:], ALU.add)
        else:
            P.dma("sp", X.vfirst[cs, :], v[:])
        P.ts(kkn[:], k[:], cols["rw_k_k"][:, ct, :])
        P.tt(t1[:], kkn[:], kkn[:], ALU.mult)
        for tt in range(4):
            ts_ = slice(tt * 512, (tt + 1) * 512)
            ps = P.psum()
            P.mm(ps[:, :], C["blk_f"][:], t1[:, ts_])
            P.act(b[:, ts_], ps[:, :], AF.Sqrt)
        P.ts(b[:], b[:], 1e-12, None, ALU.max)
        P.recip(b[:], b[:])
        P.tt(kkn[:], kkn[:], b[:], ALU.mult)
        P.ts(t1[:], a[:], cols["rw_k_a"][:, ct, :], omka[:, ct, :], ALU.mult, ALU.add)
        P.tt(k[:], k[:], t1[:], ALU.mult)
        P.tt(b[:], kkn[:], a[:], ALU.mult)
        P.stt(t1[:], r[:], cols["rk"][:, ct, :], k[:], ALU.mult, ALU.mult)
        for tt in range(4):
            ts_ = slice(tt * 512, (tt + 1) * 512)
            ps = P.psum()
            P.mm(ps[:, :], C["blk_f"][:], t1[:, ts_])
            P.tt(bonus[:, ts_], ps[:, :], v[:, ts_], ALU.mult)
        P.copy(Vb[:], v[:], eng="act")
        P.op("dve", lambda e: e.tensor_tensor_scan(out=csn[:], data0=rmask[:], data1=e2[:], initial=0.0,
                                                   op0=ALU.mult, op1=ALU.add), reads=[rmask, e2], writes=[csn])
        P.act(t1[:], csn[:], AF.Exp, scale=-1.0)
        P.tt(Rt[:], r[:], t1[:], ALU.mult)
        P.act(t1[:], csn[:], AF.Exp)
        P.tt(Kt[:], k[:], t1[:], ALU.mult)
        P.tt(Bt[:], b[:], t1[:], ALU.mult)
        P.tt(t1[:], csn[:], e2[:], ALU.subtract)
        P.act(t1[:], t1[:], AF.Exp, scale=-1.0)
        P.stt(At[:], kkn[:], -1.0, t1[:], ALU.mult, ALU.mult)
        totn = csn[:, 63:T:64]
        P.act(gam[:], totn, AF.Exp, scale=-1.0)
        csn3 = csn[:].rearrange("p (c s) -> p c s", s=64)
        t13 = t1[:].rearrange("p (c s) -> p c s", s=64)
        P.tt(t13, totn.unsqueeze(2).to_broadcast([128, 32, 64]), csn3, ALU.subtract)
        P.act(t1[:], t1[:], AF.Exp, scale=-1.0)
        P.tt(Kh[:], k[:], t1[:], ALU.mult)
        P.tt(Bh[:], b[:], t1[:], ALU.mult)
        P.release(mp)
        chk(X, 2)

        ytok = P.alloc("ytok", [128, 32, 64], F32, split=True)
        mq = P.mark()
        Vtok = P.alloc("Vtok", [128, 32, 64], BF16, split=True)
        Khtok = P.alloc("Khtok", [128, 32, 64], BF16, split=True)
        Bhtok = P.alloc("Bhtok", [128, 32, 64], BF16, split=True)
        IM = P.alloc("IM", [128, 32, 192], BF16, split=True)
        TTg = [P.alloc("TT%d" % g, [128, 4, 64], BF16, split=True) for g in range(8)]
        LU = [[P.alloc("LU%d_%d" % (s, pp), [128, 4, 128], BF16, split=True) for pp in range(2)] for s in range(2)]
        PB = [slice(0, 64), slice(64, 128)]
        for src, dst in ((Vb, Vtok), (Kh, Khtok), (Bh, Bhtok)):
            for c8 in range(4):
                for hh in range(2):
                    pb = PB[hh]
                    ps = P.psum()
                    for ci in range(8):
                        c = c8 * 8 + ci
                        P.mm(ps[pb, ci * 64:(ci + 1) * 64], src[pb, c * 64:(c + 1) * 64], C["ident_b"][pb, pb])
                    P.copy(dst[pb, c8 * 8:(c8 + 1) * 8, :].rearrange("p a b -> p (a b)"), ps[pb, :],
                           eng=("act" if hh else "dve"))
        chk(X, 3)
        for c2 in range(16):
            for hh in range(2):
                pb = PB[hh]
                ps = P.psum()
                for ci in range(2):
                    c = c2 * 2 + ci
                    sl = slice(c * 64, (c + 1) * 64)
                    o = ci * 192
                    P.mm(ps[pb, o:o + 64], Kt[pb, sl], At[pb, sl])
                    P.mm(ps[pb, o + 64:o + 128], Bt[pb, sl], Rt[pb, sl])
                    P.mm(ps[pb, o + 128:o + 192], Kt[pb, sl], Rt[pb, sl])
                P.tt(IM[pb, c2 * 2:c2 * 2 + 2, :].rearrange("p a b -> p (a b)"), ps[pb, 0:384], rwm3[pb, :], ALU.mult)
        chk(X, 4)
        for gp in range(4):
            grp = [gp * 2, gp * 2 + 1]
            comb = [(s, g, hh) for s, g in enumerate(grp) for hh in range(2)]
            for s, g, hh in comb:
                pb = PB[hh]
                ps = P.psum()
                for ci in range(4):
                    c = g * 4 + ci
                    sl = slice(c * 64, (c + 1) * 64)
                    o = ci * 128
                    P.mm(ps[pb, o:o + 64], At[pb, sl], Bt[pb, sl])
                    P.mm(ps[pb, o + 64:o + 128], Bt[pb, sl], At[pb, sl])
                P.tt(LU[s][0][pb].rearrange("p a b -> p (a b)"), ps[pb, :], rwm2[pb, :], ALU.mult)
                P.tt(TTg[g][pb], LU[s][0][pb, :, 64:128], identbc[pb].rearrange("p (a b) -> p a b", a=4), ALU.add)
            for lev in range(1, 6):
                pi, po = (lev - 1) % 2, lev % 2
                psl = {}
                for s, g, hh in comb:
                    pb = PB[hh]
                    ps = P.psum()
                    psl[(s, hh)] = ps
                    for ci in range(4):
                        o = ci * 128
                        Lp = LU[s][pi][pb, ci, 0:64]
                        Up = LU[s][pi][pb, ci, 64:128]
                        P.mm(ps[pb, o:o + 64], Up, Lp)
                        P.mm(ps[pb, o + 64:o + 128], Lp, Up)
                    if (s, hh) == (0, 1) or (s, hh) == (1, 1):
                        for hh2 in range(2):
                            P.copy(LU[s][po][PB[hh2]].rearrange("p a b -> p (a b)"), psl[(s, hh2)][PB[hh2], :],
                                   eng=("act" if hh2 else "dve"))
                pst = {}
                for s, g, hh in comb:
                    pb = PB[hh]
                    ps = P.psum()
                    pst[(s, hh)] = ps
                    for ci in range(4):
                        P.mm(ps[pb, ci * 64:(ci + 1) * 64], LU[s][po][pb, ci, 0:64], TTg[g][pb, ci, :])
                for s, g, hh in comb:
                    pb = PB[hh]
                    P.tt(TTg[g][pb].rearrange("p a b -> p (a b)"), TTg[g][pb].rearrange("p a b -> p (a b)"),
                         pst[(s, hh)][pb, 0:256], ALU.add)
        chk(X, 5)
        Pst = P.alloc("Pst", [128, 64], F32, split=True)
        Pbf = P.alloc("Pbf", [128, 64], BF16, split=True)
        P.memset(Pst[:], 0.0)
        P.memset(Pbf[:], 0.0)
        r1s = [P.alloc("r1_%d" % i, [128, 64], BF16, split=True) for i in range(2)]
        Us = [P.alloc("U_%d" % i, [128, 64], BF16, split=True) for i in range(2)]
        for c in range(32):
            sl = slice(c * 64, (c + 1) * 64)
            g, ci = c // 4, c % 4
            r1 = r1s[c % 2]; U = Us[c % 2]
            for hh in range(2):
                pb = PB[hh]
                ps = P.psum()
                P.mm(ps[pb, 0:64], IM[pb, c, 0:64], Vtok[pb, c, :], start=True, stop=False)
                P.mm(ps[pb, 0:64], At[pb, sl], Pbf[pb, :], start=False, stop=True)
                P.copy(r1[pb, :], ps[pb, 0:64], eng=("act" if hh else "dve"))
            for hh in range(2):
                pb = PB[hh]
                ps = P.psum()
                P.mm(ps[pb, 0:64], TTg[g][pb, ci, :], r1[pb, :])
                P.copy(U[pb, :], ps[pb, 0:64], eng=("act" if hh else "dve"))
            for hh in range(2):
                pb = PB[hh]
                ps = P.psum()
                P.mm(ps[pb, 0:64], Bhtok[pb, c, :], U[pb, :], start=True, stop=False)
                P.mm(ps[pb, 0:64], Khtok[pb, c, :], Vtok[pb, c, :], start=False, stop=True)
                psy = P.psum()
                P.mm(psy[pb, 0:64], Rt[pb, sl], Pbf[pb, :], start=True, stop=False)
                P.mm(psy[pb, 0:64], IM[pb, c, 64:128], U[pb, :], start=False, stop=False)
                P.mm(psy[pb, 0:64], IM[pb, c, 128:192], Vtok[pb, c, :], start=False, stop=True)
                P.stt(Pbf[pb, :], Pst[pb, :], gam[pb, c:c + 1], ps[pb, 0:64], ALU.mult, ALU.add)
                P.stt(Pst[pb, :], Pst[pb, :], gam[pb, c:c + 1], ps[pb, 0:64], ALU.mult, ALU.add)
                P.copy(ytok[pb, c, :], psy[pb, 0:64], eng=("act" if hh else "dve"))
        chk(X, 6)
        P.release(mq)
        sq = P.alloc("gsq", [128, 32, 64], F32)
        s1 = P.alloc("gs1", [128, 32], F32)
        s2 = P.alloc("gs2", [128, 32], F32)
        yv = ytok[:]
        P.tt(sq[:], yv, yv, ALU.mult)
        P.op("dve", lambda e: e.tensor_reduce(out=s1[:], in_=yv, axis=AX.X, op=ALU.add), reads=[ytok], writes=[s1])
        P.op("dve", lambda e: e.tensor_reduce(out=s2[:], in_=sq[:], axis=AX.X, op=ALU.add), reads=[sq], writes=[s2])
        P.ts(s1[:], s1[:], 1.0 / 64)
        P.ts(s2[:], s2[:], 1.0 / 64)
        m2_ = P.alloc("gm2", [128, 32], F32)
        P.tt(m2_[:], s1[:], s1[:], ALU.mult)
        P.tt(s2[:], s2[:], m2_[:], ALU.subtract)
        P.act(s2[:], s2[:], AF.Sqrt, bias=C["gneps"][:])
        P.recip(s2[:], s2[:])
        P.tt(yv, yv, s1[:].unsqueeze(2).to_broadcast([128, 32, 64]), ALU.subtract)
        P.tt(yv, yv, s2[:].unsqueeze(2).to_broadcast([128, 32, 64]), ALU.mult)
        yo = P.alloc("yo", [128, T], F32, split=True)
        for c8 in range(4):
            for hh in range(2):
                pb = PB[hh]
                ps = P.psum()
                for ci in range(8):
                    c = c8 * 8 + ci
                    P.mm(ps[pb, ci * 64:(ci + 1) * 64], ytok[pb, c, :], C["ident_f"][pb, pb])
                P.ts(yo[pb, c8 * 512:(c8 + 1) * 512], ps[pb, :], cols["rw_ln_w"][pb, ct, :], cols["rw_ln_b"][pb, ct, :],
                     ALU.mult, ALU.add)
        P.tt(yo[:], yo[:], bonus[:], ALU.add)
        ob = P.alloc("ob", [128, T], BF16)
        P.tt(ob[:], yo[:], gT[:], ALU.mult)
        P.dma("sp", X.ymix[cs, :], ob[:])
        if X.dbg is not None and "yrw" in X.dbg:
            P.tt(yo[:], yo[:], gT[:], ALU.mult)
            P.dma("sp", X.dbg["yrw"][cs, :], yo[:])
        P.release(mc)
        chk(X, 7)
    P.release(m0)


def nsa_phase(P, C, X, L):
    cin = X.cin
    inp = X.inp
    n_rw = X.n_rw
    m0 = P.mark()
    def ctab(name, shape, dt=BF16, q="pool"):
        b = P.alloc(name, shape, dt)
        P.dma(q, b[:], cin[name])
        return b
    cneg = ctab("cneg", [128, 2048])
    wneg = ctab("wneg", [128, 2048])
    cmpneg = ctab("cmpneg", [128, 2048])
    eexp = P.alloc("eexp", [128, 2048], BF16)
    P.memset(eexp[:], 0.0, eng="dve")
    P.dma("pool", eexp[0:32, :], cin["eexp"])
    bias_sw = ctab("bias_sw", [128, 128], F32, "sp")
    bias_cmp = ctab("bias_cmp", [128, 32], F32, "sp")
    topA = ctab("topA", [128, 256], F32, "sp")
    topB = ctab("topB", [128, 256], F32, "sp")
    qa = [P.alloc("qa%d" % h, [128, T], BF16) for h in range(8)]
    ka = {}
    for br in (1, 2):
        for g in range(2):
            ka[(br, g)] = P.alloc("ka%d%d" % (br, g), [128, T], BF16)
    vtok = {}
    for br in (1, 2):
        for g in range(2):
            vtok[(br, g)] = P.alloc("vtok%d%d" % (br, g), [128, 16, 66], BF16)
    kc_a = [P.alloc("kc_a%d" % g, [128, 128], BF16) for g in range(2)]
    vc_aug = [P.alloc("vc_aug%d" % g, [128, 98], BF16) for g in range(2)]
    gates_tok = P.alloc("gates_tok", [128, 16, 24], F32)
    ynsa = P.alloc("ynsa", [128, 16, 512], F32)
    imp = P.alloc("imp", [128, 8, 2, 32], F32)
    negselT = [P.alloc("negselT%d" % g, [128, T], BF16) for g in range(2)]
    for h in range(8):
        P.memset(qa[h][:], 0.0, eng="dve")
        P.dma("pool", qa[h][64:66, :], cin["alibi_q"][2 * h:2 * h + 2, :])
    for kk_ in ka.values():
        P.memset(kk_[:], 0.0, eng="dve")
        P.memset(kk_[64:66, :], 1.0, eng="dve")
    for g in range(2):
        P.memset(kc_a[g][:], 0.0, eng="dve")
        P.memset(kc_a[g][64:66, :], 1.0, eng="dve")
        P.memset(vc_aug[g][:], 0.0, eng="dve")
        P.memset(vc_aug[g][:, 64:65], 1.0, eng="dve")
        P.dma("pool", vc_aug[g][:, 65:97], cin["ov"])
        P.memset(negselT[g][:], 0.0, eng="dve")
    for vt in vtok.values():
        P.memset(vt[:, :, 64:65], 1.0, eng="dve")
    wsbl = [P.alloc("wsbn%d" % i, [128, 8, 64], BF16) for i in range(3)]
    wsi = [0]

    def proj_piece(c0, n, epi):
        wsb = wsbl[wsi[0] % 3]
        wsi[0] += 1
        load_w(P, wsb, X.w_in, n_rw + c0, n)
        for tt in range(4):
            ps = proj_psum(P, X, wsb, n, tt)
            epi(tt, ps)

    for h in range(8):
        proj_piece(h * 64, 64, lambda tt, ps, h=h: P.act(qa[h][0:64, tt * 512:(tt + 1) * 512], ps[0:64, :], AF.Copy, scale=0.125))
    for br in (1, 2):
        for g in range(2):
            c0 = 512 + (2 * br) * 128 + g * 64
            proj_piece(c0, 64, lambda tt, ps, br=br, g=g: P.copy(ka[(br, g)][0:64, tt * 512:(tt + 1) * 512], ps[0:64, :], eng="act"))
    mt = P.mark()
    vT = P.alloc("vT", [64, T], BF16)
    for br in (1, 2):
        for g in range(2):
            c0 = 512 + (2 * br + 1) * 128 + g * 64
            proj_piece(c0, 64, lambda tt, ps: P.copy(vT[0:64, tt * 512:(tt + 1) * 512], ps[0:64, :], eng="act"))
            for t8 in range(2):
                ps = P.psum()
                for ti in range(8):
                    tile = t8 * 8 + ti
                    P.mm(ps[:, ti * 64:(ti + 1) * 64], vT[0:64, tile * 128:(tile + 1) * 128], C["ident_b"][0:64, 0:64])
                P.copy(vtok[(br, g)][:, t8 * 8:(t8 + 1) * 8, 0:64], ps[:, :].rearrange("p (a b) -> p a b", a=8), eng="dve")
    gsT = P.alloc("gsT", [24, T], F32)
    proj_piece(1280, 24, lambda tt, ps: P.act(gsT[0:24, tt * 512:(tt + 1) * 512], ps[0:24, :], AF.Sigmoid))
    ps = P.psum()
    for tile in range(16):
        P.mm(ps[:, tile * 24:(tile + 1) * 24], gsT[0:24, tile * 128:(tile + 1) * 128], C["ident_f"][0:24, 0:24])
    P.copy(gates_tok[:].rearrange("p a b -> p (a b)"), ps[:, 0:384], eng="dve")
    kcT = [P.alloc("kcT%d" % g, [64, T], BF16) for g in range(2)]
    w1 = P.alloc("w1", [64, 32, 128], BF16)
    peT = P.alloc("peT", [64, 32, 2], BF16)
    w2 = P.alloc("w2", [128, 64], BF16)
    bvec = P.alloc("bvec", [128, 1], F32)
    xs = P.alloc("xs", [128, 128], F32)
    x2 = P.alloc("x2", [128, 128], F32)
    gl = P.alloc("gl", [128, 128], BF16)
    for kind in range(2):
        nm = "cmp_k" if kind == 0 else "cmp_v"
        for g in range(2):
            c0 = 512 + kind * 128 + g * 64
            proj_piece(c0, 64, lambda tt, ps, g=g: P.copy(kcT[g][0:64, tt * 512:(tt + 1) * 512], ps[0:64, :], eng="act"))
        P.dma("pool", w1[:], inp[nm + "_w1"][L].rearrange("(l d) h -> d l h", d=64))
        for j in range(2):
            P.dma("pool", peT[:, :, j:j + 1], inp[nm + "_pe"][L].rearrange("l (d o) -> d l o", o=1), allow_slow_non_contiguous=True)
        P.dma("pool", w2[:], inp[nm + "_w2"][L])
        ps = P.psum()
        for l in range(32):
            P.mm(ps[:, 0:2], w1[:, l, :], peT[:, l, :], start=(l == 0), stop=(l == 31))
        P.copy(bvec[:], ps[:, 0:1], eng="act")
        for g in range(2):
            ps = P.psum()
            for l in range(32):
                P.mm(ps[:, 0:127], w1[:, l, :], kcT[g][0:64, l:l + 16 * 126 + 1:16], start=(l == 0), stop=(l == 31))
            P.act(xs[:, 0:127], ps[:, 0:127], AF.Identity, bias=bvec[:])
            P.tt(x2[:, 0:127], xs[:, 0:127], xs[:, 0:127], ALU.mult)
            P.ts(x2[:, 0:127], x2[:, 0:127], 0.044715, 1.0, ALU.mult, ALU.add)
            P.tt(x2[:, 0:127], x2[:, 0:127], xs[:, 0:127], ALU.mult)
            P.act(x2[:, 0:127], x2[:, 0:127], AF.Sigmoid, scale=1.5957691216057308)
            P.tt(gl[:, 0:127], xs[:, 0:127], x2[:, 0:127], ALU.mult)
            ps = P.psum()
            if kind == 0:
                P.mm(ps[0:64, 0:127], w2[:, :], gl[:, 0:127])
                P.copy(kc_a[g][0:64, 0:127], ps[0:64, 0:127], eng="act")
            else:
                P.mm(ps[0:127, 0:64], gl[:, 0:127], w2[:, :])
                P.copy(vc_aug[g][0:127, 0:64], ps[0:127, 0:64], eng="act")
    P.release(mt)
    chk(X, 11)

    eTs = [P.alloc("eT%d" % i, [128, 512], BF16) for i in range(3)]
    eti = [0]
    rsb = P.alloc("rsb", [128, 8], F32)
    rsi = [0]

    def next_eT():
        e = eTs[eti[0] % 3]
        eti[0] += 1
        return e

    def epilogue(acc, m, h, br, first):
        i = rsi[0] % 4
        rsi[0] += 1
        rs = rsb[:, 2 * i:2 * i + 1]
        cf = rsb[:, 2 * i + 1:2 * i + 2]
        P.ts(rs, acc[:, 64:65], 1e-30, None, ALU.max)
        P.recip(rs, rs)
        P.tt(cf, rs, gates_tok[:, m, h * 3 + br:h * 3 + br + 1], ALU.mult)
        dst = ynsa[:, m, h * 64:(h + 1) * 64]
        if first:
            P.ts(dst, acc[:, 0:64], cf, None, ALU.mult)
        else:
            P.stt(dst, acc[:, 0:64], cf, dst, ALU.mult, ALU.add)
        return rs

    rs4 = P.alloc("rs4", [128, 2, 4], F32)
    cf4 = P.alloc("cf4", [128, 2, 4], F32)
    ei = 0
    for h in range(8):
        g, r = h // 4, h % 4
        for tt in range(4):
            ts_ = slice(tt * 512, (tt + 1) * 512)
            ps = P.psum()
            P.mm(ps[0:127, :], kc_a[g][:, 0:127], qa[h][:, ts_], start=True, stop=False)
            P.mm(ps[0:127, :], C["ident_b"][:, 0:127], cmpneg[:, ts_], start=False, stop=True)
            eT = next_eT()
            P.act(eT[0:127, :], ps[0:127, :], AF.Exp, bias=bias_cmp[0:127, h * 4 + tt:h * 4 + tt + 1])
            acc = P.psum()
            for sub in range(4):
                P.mm(acc[:, sub * 97:(sub + 1) * 97], eT[0:127, sub * 128:(sub + 1) * 128], vc_aug[g][0:127, 0:97])
            av = acc[:, 0:388].rearrange("p (a b) -> p a b", a=4)
            rs = rs4[:, ei % 2, :]
            cf = cf4[:, ei % 2, :]
            ei += 1
            P.ts(rs, av[:, :, 64], 1e-30, None, ALU.max)
            P.recip(rs, rs)
            P.tt(cf, rs, gates_tok[:, 4 * tt:4 * tt + 4, h * 3], ALU.mult)
            P.tt(ynsa[:, 4 * tt:4 * tt + 4, h * 64:(h + 1) * 64], av[:, :, 0:64],
                 cf.unsqueeze(2).to_broadcast([128, 4, 64]), ALU.mult)
            if tt >= 2:
                iv = imp[:, 4 * (tt - 2):4 * (tt - 2) + 4, g, :]
                if r == 0:
                    P.tt(iv, av[:, :, 65:97], rs.unsqueeze(2).to_broadcast([128, 4, 32]), ALU.mult)
                else:
                    tmp4 = P.alloc("imptmp%d_%d" % (h, tt), [128, 4, 32], F32)
                    P.tt(tmp4[:], av[:, :, 65:97], rs.unsqueeze(2).to_broadcast([128, 4, 32]), ALU.mult)
                    P.tt(iv, iv, tmp4[:], ALU.add)
    chk(X, 12)
    mx = P.alloc("mx", [128, 16], F32)
    wk_ = P.alloc("wk", [128, 32], F32)
    sel = P.alloc("sel", [128, 32], F32)
    nsb = P.alloc("nsb", [128, 32], BF16)
    for mi in range(8):
        for g in range(2):
            iv = imp[:, mi, g, :]
            P.tt(iv, iv, topA[:, mi * 32:(mi + 1) * 32], ALU.mult)
            P.tt(iv, iv, topB[:, mi * 32:(mi + 1) * 32], ALU.add)
            P.op("dve", lambda e, iv=iv: e.max(out=mx[:, 0:8], in_=iv), reads=[imp], writes=[mx])
            P.op("dve", lambda e, iv=iv: e.match_replace(out=wk_[:], in_to_replace=mx[:, 0:8], in_values=iv, imm_value=-2.0),
                 reads=[imp, mx], writes=[wk_])
            P.op("dve", lambda e: e.max(out=mx[:, 8:16], in_=wk_[:]), reads=[wk_], writes=[mx])
            P.ts(sel[:], iv, mx[:, 15:16], None, ALU.is_ge)
            P.ts(nsb[:], sel[:], -NEG, NEG, ALU.mult, ALU.add)
            ps = P.psum()
            P.mm(ps[0:32, 0:128], nsb[:, :], C["ident_b"][:, :])
            P.copy(negselT[g][0:32, (8 + mi) * 128:(9 + mi) * 128], ps[0:32, 0:128], eng="act")
    chk(X, 13)
    sb_i = [0]
    accs = [P.banks[i] for i in range(4)]
    jobs = []
    for h in range(8):
        for tt in range(4):
            for br in (1, 2):
                kt0 = 0 if br == 1 else max(0, 4 * tt - 4)
                for kt in range(kt0, 4 * tt + 4):
                    jobs.append((h, tt, br, kt, kt == 4 * tt + 3))

    def emit_score(job):
        h, tt, br, kt, last = job
        g = h // 4
        t0 = tt * 512
        ps = P.banks[4 + sb_i[0] % 4]
        sb_i[0] += 1
        c0, c1, mk = 0, 512, None
        if kt >= 4 * tt:
            o = kt - 4 * tt
            c0 = 128 * o
            mk = (cneg[:, 0:128], 128 * o)
        elif br == 2:
            o = kt - (4 * tt - 4)
            c1 = 128 * (o + 1)
            mk = (wneg[:, 0:128], 128 * o)
        mms = [(ka[(br, g)][:, kt * 128:(kt + 1) * 128], qa[h][:, t0 + c0:t0 + c1], c0, c1)]
        if br == 1 and tt >= 2:
            mms.append((eexp[:, kt * 128:(kt + 1) * 128], negselT[g][:, t0 + c0:t0 + c1], c0, c1))
        if mk is not None:
            mms.append((C["ident_b"][:, :], mk[0], mk[1], mk[1] + 128))
        for i, (l_, r_, a0, a1) in enumerate(mms):
            P.mm(ps[:, a0:a1], l_, r_, start=(i == 0), stop=(i == len(mms) - 1))
        eT = next_eT()
        bidx = h * 16 + (4 * tt - kt + 3)
        P.act(eT[:, c0:c1], ps[:, c0:c1], AF.Exp, bias=bias_sw[:, bidx:bidx + 1])
        return eT

    def emit_pv(job, eT):
        h, tt, br, kt, last = job
        g = h // 4
        for sub in range(4):
            hi = 4 * tt + sub
            lo = 0 if br == 1 else max(0, hi - 4)
            if lo <= kt <= hi:
                P.mm(accs[sub][:, 0:65], eT[:, sub * 128:(sub + 1) * 128], vtok[(br, g)][:, kt, 0:65],
                     start=(kt == lo), stop=(kt == hi))
        if last:
            for sub in range(4):
                epilogue(accs[sub], 4 * tt + sub, h, br, False)

    pend = None
    for job in jobs:
        eT = emit_score(job)
        if pend is not None:
            emit_pv(*pend)
        pend = (job, eT)
    emit_pv(*pend)
    chk(X, 14)
    stg = [P.alloc("nstg%d" % i, [128, 512], BF16) for i in range(2)]
    si = 0
    for cg in range(4):
        for m4 in range(4):
            ps = P.psum()
            for j in range(4):
                m = m4 * 4 + j
                P.mm(ps[:, j * 128:(j + 1) * 128], ynsa[:, m, cg * 128:(cg + 1) * 128], C["ident_f"][:, :])
            s = stg[si % 2]
            si += 1
            P.copy(s[:], ps[:, :], eng="act")
            P.dma("sp", X.ymix[512 + cg * 128:512 + (cg + 1) * 128, m4 * 512:(m4 + 1) * 512], s[:], wk=[("ymixn", cg, m4)])
            if X.dbg is not None and "ynsa" in X.dbg:
                if not hasattr(X, "_dbg32"):
                    X._dbg32 = [P.alloc("dbgs%d" % i, [128, 512], F32) for i in range(2)]
                s32 = X._dbg32[si % 2]
                P.copy(s32[:], ps[:, :], eng="dve")
                P.dma("sp", X.dbg["ynsa"][cg * 128:(cg + 1) * 128, m4 * 512:(m4 + 1) * 512], s32[:])
    P.release(m0)


def resid_proj(P, C, X, actT, W, KC, x_src, x_dst, akey=None):
    m = P.mark()
    wo = P.alloc("wo", [128, KC, 1024], BF16)
    for q4 in range(4):
        P.dma("pool", wo[:, :, q4 * 256:(q4 + 1) * 256], W[:, q4 * 256:(q4 + 1) * 256].rearrange("(c p) n -> p c n", p=128),
              wk=[(wo.key, q4)])
    xts = [P.alloc("rxt%d" % i, [128, 512], F32) for i in range(4)]
    i = 0
    for dc in range(8):
        for tt in range(4):
            ts_ = slice(tt * 512, (tt + 1) * 512)
            xt = xts[i % 4]
            i += 1
            P.dma("sp", xt[:], x_src[dc * 128:(dc + 1) * 128, ts_], rk=[("xsrc", dc, tt)])
            ps = P.psum()
            for kc in range(KC):
                P.mm(ps[:, :], wo[:, kc, dc * 128:(dc + 1) * 128], actT[:, kc, ts_], start=(kc == 0), stop=(kc == KC - 1),
                     rk=[(wo.key, dc // 2), (actT[:, kc, ts_] if akey is None else akey(kc))])
            P.tt(xt[:], xt[:], ps[:, :], ALU.add)
            P.dma("sp", x_dst[dc * 128:(dc + 1) * 128, ts_], xt[:], wk=[("xsrc", dc, tt)])
    P.release(m)


def wout_phase(P, C, X, L, x_src, x_dst):
    m = P.mark()
    ym = P.alloc("ym", [128, 8, T], BF16)
    for q4 in range(4):
        P.dma("sp", ym[:, q4 * 2:q4 * 2 + 2, :], X.ymix[q4 * 256:(q4 + 1) * 256, :].rearrange("(c p) t -> p c t", p=128),
              wk=[(ym.key, q4)])
    resid_proj(P, C, X, ym, X.inp["w_out"][L], 8, x_src, x_dst, akey=lambda kc: (ym.key, kc // 2))
    P.release(m)


def cross_phase(P, C, X, L, x_src, x_dst):
    inp = X.inp
    m0 = P.mark()
    hT = P.alloc("hTc", [128, 8, T], BF16)
    rmsnorm_T(P, C, x_src, inp["cross_norm_g"][L], hT, T)
    mTn = P.alloc("mTn", [128, 8, 256], BF16)
    rmsnorm_T(P, C, X.memT, inp["mem_norm_g"][L], mTn, 256)
    KT = P.alloc("KT", [128, 8, 256], BF16)
    Vtok = P.alloc("Vtokc", [128, 2, 1024], BF16)
    qT = P.alloc("qT", [128, 8, T], BF16)
    oT = P.alloc("oT", [128, 8, T], BF16)
    wkv = inp["cross_wkv"][L]
    wq = inp["cross_wq"][L]
    m1 = P.mark()
    wsbs = [P.alloc("wsbc%d" % i, [128, 8, 128], BF16) for i in range(2)]
    wv = P.alloc("wv", [128, 8, 1024], BF16)
    for q4 in range(4):
        P.dma("pool", wv[:, :, q4 * 256:(q4 + 1) * 256],
              wkv[:, 1024 + q4 * 256:1024 + (q4 + 1) * 256].rearrange("(c p) n -> p c n", p=128), wk=[(wv.key, q4)])
    for cg in range(8):
        wsb = wsbs[cg % 2]
        load_w(P, wsb, wkv, cg * 128, 128)
        ps = P.psum()
        for dc in range(8):
            P.mm(ps[:, 0:256], wsb[:, dc, :], mTn[:, dc, :], start=(dc == 0), stop=(dc == 7))
        P.copy(KT[:, cg, :], ps[:, 0:256], eng="act")
    for mt in range(2):
        for vg in range(2):
            ps = P.psum()
            for dc in range(8):
                P.mm(ps[:, :], mTn[:, dc, mt * 128:(mt + 1) * 128], wv[:, dc, vg * 512:(vg + 1) * 512],
                     start=(dc == 0), stop=(dc == 7), rk=[mTn[:, dc, :], (wv.key, 2 * vg), (wv.key, 2 * vg + 1)])
            P.copy(Vtok[:, mt, vg * 512:(vg + 1) * 512], ps[:, :], eng="act")
    for cg in range(8):
        wsb = wsbs[cg % 2]
        load_w(P, wsb, wq, cg * 128, 128)
        for tt in range(4):
            ps = P.psum()
            for dc in range(8):
                P.mm(ps[:, :], wsb[:, dc, :], hT[:, dc, tt * 512:(tt + 1) * 512], start=(dc == 0), stop=(dc == 7))
            P.copy(qT[:, cg, tt * 512:(tt + 1) * 512], ps[:, :], eng="act")
    P.release(m1)
    eTs = [P.alloc("eTc%d" % i, [128, 2, 512], BF16) for i in range(2)]
    rsbs = [P.alloc("rsbc%d" % i, [128, 512], F32) for i in range(2)]
    it = 0
    for hd in range(4):
        for tt in range(4):
            ts_ = slice(tt * 512, (tt + 1) * 512)
            eT = eTs[it % 2]; rsb = rsbs[it % 2]
            it += 1
            for mt in range(2):
                ps = P.psum()
                for sub in range(2):
                    P.mm(ps[:, :], KT[:, hd * 2 + sub, mt * 128:(mt + 1) * 128], qT[:, hd * 2 + sub, ts_],
                         start=(sub == 0), stop=(sub == 1))
                P.act(eT[:, mt, :], ps[:, :], AF.Exp, scale=1.0 / 16.0)
            ps = P.psum()
            for mt in range(2):
                P.mm(ps[:, :], C["ones_b"][:, :], eT[:, mt, :], start=(mt == 0), stop=(mt == 1))
            P.recip(rsb[:], ps[:, :])
            for ds in range(2):
                ps = P.psum()
                for mt in range(2):
                    P.mm(ps[:, :], Vtok[:, mt, hd * 256 + ds * 128:hd * 256 + (ds + 1) * 128], eT[:, mt, :],
                         start=(mt == 0), stop=(mt == 1))
                P.tt(oT[:, hd * 2 + ds, ts_], ps[:, :], rsb[:], ALU.mult)
    resid_proj(P, C, X, oT, inp["cross_wo"][L], 8, x_src, x_dst)
    P.release(m0)


def ffn_core(P, C, X, hT, xacc, wg, wu, wd, F, bufs, gbc=None):
    nfc = F // 128
    GS = 4
    wgs, wus, wds, actTs, t1s, t2s = bufs
    gi = 0
    for f0 in range(0, nfc, GS):
        grp = list(range(f0, min(nfc, f0 + GS)))
        actT = actTs[gi % 2]; wdb = wds[gi % 2]
        gi += 1
        P.dma("pool", wdb[:, 0:len(grp), :], wd[f0 * 128:(f0 + len(grp)) * 128, :].rearrange("(c p) n -> p c n", p=128))
        for j, fc in enumerate(grp):
            wgb = wgs[fc % 2]; wub = wus[fc % 2]
            load_w(P, wgb, wg, fc * 128, 128)
            load_w(P, wub, wu, fc * 128, 128)
            for tt in range(4):
                ts_ = slice(tt * 512, (tt + 1) * 512)
                psg = P.psum()
                for dc in range(8):
                    P.mm(psg[:, :], wgb[:, dc, :], hT[:, dc, ts_], start=(dc == 0), stop=(dc == 7))
                psu = P.psum()
                for dc in range(8):
                    P.mm(psu[:, :], wub[:, dc, :], hT[:, dc, ts_], start=(dc == 0), stop=(dc == 7))
                t1 = t1s[tt % 2]
                P.act(t1[:], psg[:, :], AF.Silu)
                if gbc is None:
                    P.tt(actT[:, j, ts_], t1[:], psu[:, :], ALU.mult)
                else:
                    t2 = t2s[tt % 2]
                    P.tt(t2[:], t1[:], psu[:, :], ALU.mult)
                    P.tt(actT[:, j, ts_], t2[:], gbc[:, ts_], ALU.mult, eng="pool")
        for dc in range(8):
            for tt in range(4):
                ts_ = slice(tt * 512, (tt + 1) * 512)
                ps = P.psum()
                for j in range(len(grp)):
                    P.mm(ps[:, :], wdb[:, j, dc * 128:(dc + 1) * 128], actT[:, j, ts_], start=(j == 0), stop=(j == len(grp) - 1))
                xk = (xacc.key, dc, tt)
                P.tt(xacc[:, dc, ts_], xacc[:, dc, ts_], ps[:, :], ALU.add, rk=[xk, ps[:, :]], wk=[xk])


def ffn_bufs(P):
    wgs = [P.alloc("wgs%d" % i, [128, 8, 128], BF16) for i in range(2)]
    wus = [P.alloc("wus%d" % i, [128, 8, 128], BF16) for i in range(2)]
    wds = [P.alloc("wds%d" % i, [128, 4, 1024], BF16) for i in range(2)]
    actTs = [P.alloc("actT%d" % i, [128, 4, T], BF16) for i in range(2)]
    t1s = [P.alloc("ft1_%d" % i, [128, 512], F32) for i in range(2)]
    t2s = [P.alloc("ft2_%d" % i, [128, 512], F32) for i in range(2)]
    return wgs, wus, wds, actTs, t1s, t2s


def load_xacc(P, xacc, x_src):
    for q4 in range(4):
        ks = [(xacc.key, dc, tt) for dc in (2 * q4, 2 * q4 + 1) for tt in range(4)]
        P.dma("sp", xacc[:, q4 * 2:q4 * 2 + 2, :], x_src[q4 * 256:(q4 + 1) * 256, :].rearrange("(c p) t -> p c t", p=128),
              rk=[x_src], wk=ks)


def store_xacc(P, xacc, x_dst):
    for q4 in range(4):
        ks = [(xacc.key, dc, tt) for dc in (2 * q4, 2 * q4 + 1) for tt in range(4)]
        P.dma("sp", x_dst[q4 * 256:(q4 + 1) * 256, :].rearrange("(c p) t -> p c t", p=128), xacc[:, q4 * 2:q4 * 2 + 2, :],
              rk=ks, wk=[x_dst])


def dense_ffn_phase(P, C, X, L, x_src, x_dst):
    inp = X.inp
    i = L // 2
    m0 = P.mark()
    hT = P.alloc("hTf", [128, 8, T], BF16)
    rmsnorm_T(P, C, x_src, inp["ffn_norm_g"][L], hT, T)
    xacc = P.alloc("xacc", [128, 8, T], F32)
    load_xacc(P, xacc, x_src)
    bufs = ffn_bufs(P)
    ffn_core(P, C, X, hT, xacc, inp["dense_wg"][i], inp["dense_wu"][i], inp["dense_wd"][i], 2816, bufs)
    store_xacc(P, xacc, x_dst)
    P.release(m0)


def moe_phase(P, C, X, L, x_src, x_dst):
    inp = X.inp
    i = L // 2
    m0 = P.mark()
    hT = P.alloc("hTm", [128, 8, T], BF16)
    gwT = P.alloc("gwT", [8, T], F32)
    oh = P.alloc("oh8", [8, 1024], F32)
    P.dma("sp", oh[:], X.cin["onehot8"])
    m1 = P.mark()
    lgT = P.alloc("lgT", [8, T], F32)
    rw = P.alloc("rw", [128, 8, 8], F32)
    P.dma("sp", rw[:], inp["router_w"][i].rearrange("(c p) e -> p c e", p=128))
    h32s = [P.alloc("h32_%d" % k, [128, 512], F32) for k in range(2)]
    st = {"ps": None, "n": 0}

    def cb(tt, dc, xt, gcol, r):
        h32 = h32s[st["n"] % 2]
        st["n"] += 1
        P.stt(h32[:], xt[:, dc, :], gcol[:, dc, :], r[:], ALU.mult, ALU.mult)
        P.copy(hT[:, dc, tt * 512:(tt + 1) * 512], h32[:], eng="act")
        if dc == 0:
            st["ps"] = P.psum()
        P.mm(st["ps"][0:8, :], rw[:, dc, :], h32[:], start=(dc == 0), stop=(dc == 7))
        if dc == 7:
            P.copy(lgT[0:8, tt * 512:(tt + 1) * 512], st["ps"][0:8, :], eng="act")

    rmsnorm_T(P, C, x_src, inp["ffn_norm_g"][L], hT, T, h32cb=cb)
    lg = P.alloc("lg", [128, 16, 8], F32)
    mx = P.alloc("mxr", [128, 16, 8], F32)
    gw = P.alloc("gw", [128, 16, 8], F32)
    tmp = P.alloc("rtmp", [128, 16, 8], F32)
    g1 = P.alloc("g1", [128, 16], F32)
    g2 = P.alloc("g2_", [128, 16], F32)
    ps = P.psum()
    for tile in range(16):
        P.mm(ps[:, tile * 8:(tile + 1) * 8], lgT[0:8, tile * 128:(tile + 1) * 128], C["ident_f"][0:8, 0:8])
    P.copy(lg[:].rearrange("p a b -> p (a b)"), ps[:, 0:128], eng="dve")
    for tile in range(16):
        P.op("dve", lambda e, tile=tile: e.max(out=mx[:, tile, :], in_=lg[:, tile, :]), reads=[lg], writes=[mx])
    P.tt(g1[:], mx[:, :, 0], mx[:, :, 1], ALU.subtract)
    P.act(g1[:], g1[:], AF.Sigmoid)
    P.ts(g2[:], g1[:], -1.0, 1.0, ALU.mult, ALU.add)
    P.tt(gw[:], lg[:], mx[:, :, 0:1].to_broadcast([128, 16, 8]), ALU.is_equal)
    P.tt(gw[:], gw[:], g1[:].unsqueeze(2).to_broadcast([128, 16, 8]), ALU.mult)
    P.tt(tmp[:], lg[:], mx[:, :, 1:2].to_broadcast([128, 16, 8]), ALU.is_equal)
    P.tt(tmp[:], tmp[:], g2[:].unsqueeze(2).to_broadcast([128, 16, 8]), ALU.mult)
    P.tt(gw[:], gw[:], tmp[:], ALU.add)
    for t4 in range(4):
        ps = P.psum()
        for j in range(4):
            tile = t4 * 4 + j
            P.mm(ps[0:8, j * 128:(j + 1) * 128], gw[:, tile, :], C["ident_f"][:, :])
        P.copy(gwT[0:8, t4 * 512:(t4 + 1) * 512], ps[0:8, :], eng="act")
    P.release(m1)
    gbc = P.alloc("gbc", [128, T], F32)
    xacc = P.alloc("xaccm", [128, 8, T], F32)
    load_xacc(P, xacc, x_src)
    bufs = ffn_bufs(P)
    for e in range(8):
        for tt in range(4):
            ps = P.psum()
            P.mm(ps[:, :], oh[0:8, e * 128:(e + 1) * 128], gwT[0:8, tt * 512:(tt + 1) * 512])
            P.copy(gbc[:, tt * 512:(tt + 1) * 512], ps[:, :], eng="act")
        ffn_core(P, C, X, hT, xacc, inp["exp_wg"][i][e], inp["exp_wu"][i][e], inp["exp_wd"][i][e], 3584, bufs, gbc=gbc)
    store_xacc(P, xacc, x_dst)
    P.release(m0)


def final_phase(P, C, X, x_src, out):
    stg = [P.alloc("fstg%d" % i, [128, 512], F32) for i in range(4)]
    st = {"n": 0}

    def cb(tt, dc, xt, gcol, r):
        s = stg[st["n"] % 4]
        st["n"] += 1
        P.stt(s[:], xt[:, dc, :], gcol[:, dc, :], r[:], ALU.mult, ALU.mult)
        P.dma("sp", out[dc * 128:(dc + 1) * 128, tt * 512:(tt + 1) * 512], s[:], wk=[("outT", dc, tt)])

    rmsnorm_T(P, C, x_src, X.inp["final_norm_g"], None, T, h32cb=cb)


PARAM_SPEC = {
    "w_in_first": [1024, 3096], "w_in_rest": [1, 1024, 3128], "shift_mu_first": [1792], "shift_mu_rest": [1, 1824],
    "mix_norm_g": [2, 1024], "rw_w0": [2, 512], "rw_w2": [2, 64, 512], "rw_a0": [2, 512], "rw_a2": [2, 64, 512],
    "rw_g2": [2, 128, 512], "rw_k_k": [2, 512], "rw_k_a": [2, 512], "rw_r_k": [2, 8, 64], "rw_ln_w": [2, 512],
    "rw_ln_b": [2, 512], "rw_v0": [1, 512], "rw_v2": [1, 32, 512], "cmp_k_pe": [2, 32, 64], "cmp_k_w1": [2, 2048, 128],
    "cmp_k_w2": [2, 128, 64], "cmp_v_pe": [2, 32, 64], "cmp_v_w1": [2, 2048, 128], "cmp_v_w2": [2, 128, 64],
    "w_out": [2, 1024, 1024], "cross_norm_g": [2, 1024], "mem_norm_g": [2, 1024], "cross_wq": [2, 1024, 1024],
    "cross_wkv": [2, 1024, 2048], "cross_wo": [2, 1024, 1024], "ffn_norm_g": [2, 1024], "dense_wg": [1, 1024, 2816],
    "dense_wu": [1, 1024, 2816], "dense_wd": [1, 2816, 1024], "router_w": [1, 1024, 8], "exp_wg": [1, 8, 1024, 3584],
    "exp_wu": [1, 8, 1024, 3584], "exp_wd": [1, 8, 3584, 1024], "final_norm_g": [1024],
}


def build_full(upto=99, dbg_names=()):
    P = Prog()
    dr = lambda name, shape, kind="ExternalInput", dt=F32: P.dram(name, shape, dt, kind).ap()
    X = Ctx()
    X.inp = {k: dr(k, v) for k, v in PARAM_SPEC.items()}
    xT = dr("xT", [D, T])
    X.memT = dr("memT", [D, 256])
    X.cin = {k: dr("c_" + k, list(v.shape)) for k, v in host_consts().items()}
    X.vfirst = dr("vfirst", [512, T], "Internal")
    X.ymix = dr("ymix", [1024, T], "Internal", BF16)
    xres = dr("xres", [D, T], "Internal")
    outT = dr("outT", [D, T], "ExternalOutput")
    X.dbg = {}
    C = load_consts(P, X.cin)
    x_cur = xT
    step = 0

    def go():
        nonlocal step
        step += 1
        return step <= upto

    for L in range(2):
        if not go():
            break
        m = P.mark()
        X.hT = P.alloc("hT", [128, 8, T], BF16)
        rmsnorm_T(P, C, x_cur, X.inp["mix_norm_g"][L], X.hT, T)
        if L == 0:
            X.w_in, X.mu, X.n_rw = X.inp["w_in_first"], X.inp["shift_mu_first"], 1792
        else:
            X.w_in, X.mu, X.n_rw = X.inp["w_in_rest"][0], X.inp["shift_mu_rest"][0], 1824
        rwkv_phase(P, C, X, L)
        nsa_phase(P, C, X, L)
        P.release(m)
        wout_phase(P, C, X, L, x_cur, xres)
        x_cur = xres
        if not go():
            break
        cross_phase(P, C, X, L, x_cur, xres)
        if not go():
            break
        if L % 2 == 0:
            dense_ffn_phase(P, C, X, L, x_cur, xres)
        else:
            moe_phase(P, C, X, L, x_cur, xres)
    if upto >= 99:
        final_phase(P, C, X, x_cur, outT)
    else:
        m = P.mark()
        xacc = P.alloc("dump", [128, 8, T], F32)
        load_xacc(P, xacc, x_cur)
        store_xacc(P, xacc, outT)
        P.release(m)
    print("ops:", {e: len(P.ops[e]) for e in P.ops})
    return P.finish()


def make_in_maps(inputs, cores):
    hc = host_consts()
    maps = []
    for b in cores:
        m = {k: np.ascontiguousarray(inputs[k], dtype=np.float32) for k in PARAM_SPEC}
        m["xT"] = np.ascontiguousarray(inputs["x"][b].T)
        m["memT"] = np.ascontiguousarray(inputs["mem"][b].T)
        for k, v in hc.items():
            m["c_" + k] = v
        maps.append(m)
    return maps


_NC = None


def kernel(**inputs):
    global _NC
    if _NC is None:
        _NC = build_full(99)
    maps = make_in_maps(inputs, list(range(8)))
    res = run_bass_kernel_spmd(_NC, maps, core_ids=list(range(8)))
    out = np.stack([np.ascontiguousarray(res.results[b]["outT"].T) for b in range(8)], 0)
    return out.astype(np.float32)
```

```python
import bisect
import numpy as np
from contextlib import ExitStack
import concourse.bass as bass
import concourse.mybir as mybir
from concourse.bass_utils import run_bass_kernel_spmd

F32 = mybir.dt.float32
BF16 = mybir.dt.bfloat16
I32 = mybir.dt.int32
U32 = mybir.dt.uint32
AF = mybir.ActivationFunctionType
ALU = mybir.AluOpType
AX = mybir.AxisListType
DSIZE = {F32: 4, BF16: 2, I32: 4, U32: 4}

ENG = ("pe", "dve", "act", "pool", "sp")
RING = {"sp": 16, "act": 4, "pool": 8}
ARENA_WORDS = 49152


class Buf:
    def __init__(self, ap, key, w0, w1):
        self.ap, self.key, self.w0, self.w1 = ap, key, w0, w1
        self.shape = tuple(ap.shape)

    def __getitem__(self, idx):
        return self.ap[idx]


class Prog:
    def __init__(self, same_engine_sync=True):
        self.nc = bass.Bass("TRN2", target_bir_lowering=False)
        self.es = ExitStack()
        self.ops = {e: [] for e in ENG}
        self.cnt = {e: 0 for e in ENG}
        self.sems = {}
        self.res = {}
        self.waited = {e: {} for e in ENG}
        self.pending = {e: [] for e in ENG}
        self.ring_pos = {q: 0 for q in RING}
        self.ring_val = {q: [0] * RING[q] for q in RING}
        self.same_engine_sync = same_engine_sync
        for e in ("pe", "dve", "act", "pool"):
            self.sems[e] = self.es.enter_context(self.nc.semaphore("s_" + e))
        for q in RING:
            for i in range(RING[q]):
                self.sems[(q, i)] = self.es.enter_context(self.nc.semaphore("r_%s_%d" % (q, i)))
        self.arena = self.es.enter_context(self.nc.sbuf_tensor("arena", [128, ARENA_WORDS], F32))
        self.top = 0
        self.allocs = []
        self.nalloc = 0
        self.banks = [self.es.enter_context(self.nc.psum_tensor("ps%d" % i, [128, 512], F32)) for i in range(8)]
        self.bank_i = 0

    def alloc(self, name, shape, dtype=F32, split=False):
        p = shape[0]
        n = int(np.prod(shape[1:]))
        words = (n * DSIZE[dtype] + 3) // 4
        words = (words + 7) // 8 * 8
        w0 = self.top
        self.top += words
        assert self.top <= ARENA_WORDS, "SBUF arena overflow at %s: %d" % (name, self.top)
        ap = self.arena[0:p, w0:w0 + words]
        if dtype != F32:
            ap = ap.bitcast(dtype)
        ap = ap[:, 0:n]
        if len(shape) > 2:
            names = " ".join("d%d" % i for i in range(len(shape) - 1))
            kw = {"d%d" % i: shape[i + 1] for i in range(len(shape) - 1)}
            ap = ap.rearrange("p (%s) -> p %s" % (names, names), **kw)
        self.nalloc += 1
        key = "%s#%d" % (name, self.nalloc)
        if split:
            key = key + "/S"
        self.allocs.append((w0, w0 + words, key))
        return Buf(ap, key, w0, w0 + words)

    def mark(self):
        return (self.top, len(self.allocs))

    def release(self, m):
        self.barrier()
        self.top = m[0]
        del self.allocs[m[1]:]

    def psum(self):
        b = self.banks[self.bank_i]
        self.bank_i = (self.bank_i + 1) % 8
        return b

    def dram(self, name, shape, dtype=F32, kind="Internal"):
        return self.nc.dram_tensor(name, list(shape), dtype, kind=kind)

    def key_of(self, ap):
        if isinstance(ap, Buf):
            if ap.key.endswith("/S"):
                return [ap.key + "lo", ap.key + "hi"]
            return ap.key
        if isinstance(ap, (str, tuple)):
            return ap
        t = ap.tensor
        if t.name == "arena":
            row = t.shape[1]
            col = ap.offset % row
            w = col * DSIZE[ap.dtype] // 4
            i = bisect.bisect_right(self.allocs, (w, 1 << 60, "")) - 1
            a = self.allocs[i]
            assert a[0] <= w < a[1], "AP outside allocations"
            if a[2].endswith("/S"):
                p0 = ap.offset // row
                p1 = p0 + ap.shape[0]
                ks = []
                if p0 < 64:
                    ks.append(a[2] + "lo")
                if p1 > 64:
                    ks.append(a[2] + "hi")
                return ks
            return a[2]
        return t.name

    def keys(self, *aps):
        out = []
        for a in aps:
            if a is None or isinstance(a, (int, float)):
                continue
            for x in (a if isinstance(a, list) else [a]):
                k = self.key_of(x)
                if isinstance(k, list):
                    out.extend(k)
                else:
                    out.append(k)
        return out

    def _deps(self, eng, reads, writes):
        need = {}

        def add(tok, hazard):
            if tok is None:
                return
            s, v, e = tok
            if e == eng:
                if eng == "pe" or not self.same_engine_sync:
                    return
            if need.get(s, (0,))[0] < v:
                need[s] = (v, e)

        for k in reads:
            st = self.res.get(k)
            if st is not None:
                add(st["w"], "raw")
        for k in writes:
            st = self.res.get(k)
            if st is not None:
                add(st["w"], "waw")
                for s, (v, e) in st["r"].items():
                    add((s, v, e), "war")
        out = list(self.pending[eng])
        self.pending[eng] = []
        wd = self.waited[eng]
        for s, v in out:
            if wd.get(s, 0) < v:
                wd[s] = v
        for s, (v, e) in need.items():
            if wd.get(s, 0) < v:
                wd[s] = v
                out.append((s, v))
        return out

    def _mark(self, tok, reads, writes):
        s, v, e = tok
        for k in reads:
            st = self.res.setdefault(k, {"w": None, "r": {}})
            st["r"][s] = (v, e)
        for k in writes:
            self.res[k] = {"w": tok, "r": {}}

    def op(self, eng, fn, reads=(), writes=()):
        reads = self.keys(*reads)
        writes = self.keys(*writes)
        waits = self._deps(eng, reads, writes)
        self.cnt[eng] += 1
        tok = (eng, self.cnt[eng], eng)
        self.ops[eng].append((waits, fn, (eng, 1)))
        self._mark(tok, reads, writes)

    def dma(self, q, out, in_, rk=None, wk=None, **kw):
        reads = self.keys(in_) if rk is None else self.keys(*rk)
        writes = self.keys(out) if wk is None else self.keys(*wk)
        pos = self.ring_pos[q]
        self.ring_pos[q] = (pos + 1) % RING[q]
        waits = self._deps(q, reads, writes)
        prev = self.ring_val[q][pos]
        s = (q, pos)
        if prev > 0 and self.waited[q].get(s, 0) < prev:
            self.waited[q][s] = prev
            waits.append((s, prev))
        val = prev + 16
        self.ring_val[q][pos] = val
        self.ops[q].append((waits, lambda e: e.dma_start(out=out, in_=in_, **kw), (s, 16)))
        self._mark((s, val, "dma"), reads, writes)

    def _all_tokens(self):
        fin = []
        for e in ("pe", "dve", "act", "pool"):
            if self.cnt[e] > 0:
                fin.append((e, self.cnt[e]))
        for q in RING:
            for i in range(RING[q]):
                if self.ring_val[q][i] > 0:
                    fin.append(((q, i), self.ring_val[q][i]))
        return fin

    def barrier(self):
        fin = self._all_tokens()
        for e in ENG:
            self.pending[e] = [(s, v) for (s, v) in fin if s != e or True]
        self.res = {}

    def mm(self, out, lhsT, rhs, start=True, stop=True, rk=None, wk=None):
        self.op("pe", lambda e: e.matmul(out, lhsT=lhsT, rhs=rhs, start=start, stop=stop),
                reads=[lhsT, rhs] if rk is None else rk, writes=[out] if wk is None else wk)

    def tr(self, out, in_, ident):
        self.op("pe", lambda e: e.transpose(out, in_, ident), reads=[in_, ident], writes=[out])

    def act(self, out, in_, func, bias=None, scale=None, accum_out=None, rk=None, wk=None):
        kw = {}
        if bias is not None:
            kw["bias"] = bias
        if scale is not None:
            kw["scale"] = scale
        if accum_out is not None:
            kw["accum_out"] = accum_out
        self.op("act", lambda e: e.activation(out=out, in_=in_, func=func, **kw),
                reads=[in_, bias, scale] if rk is None else rk,
                writes=[out, accum_out] if wk is None else wk)

    def tt(self, out, in0, in1, op, eng="dve", rk=None, wk=None):
        self.op(eng, lambda e: e.tensor_tensor(out=out, in0=in0, in1=in1, op=op),
                reads=[in0, in1] if rk is None else rk, writes=[out] if wk is None else wk)

    def ts(self, out, in0, s1, s2=None, op0=ALU.mult, op1=None, eng="dve", accum_out=None, rk=None, wk=None):
        kw = {}
        if op1 is not None:
            kw["op1"] = op1
        if accum_out is not None:
            kw["accum_out"] = accum_out
        self.op(eng, lambda e: e.tensor_scalar(out=out, in0=in0, scalar1=s1, scalar2=s2, op0=op0, **kw),
                reads=[in0, s1, s2] if rk is None else rk, writes=[out, accum_out] if wk is None else wk)

    def stt(self, out, in0, scalar, in1, op0, op1, rk=None, wk=None):
        self.op("dve", lambda e: e.scalar_tensor_tensor(out=out, in0=in0, scalar=scalar, in1=in1, op0=op0, op1=op1),
                reads=[in0, scalar, in1] if rk is None else rk, writes=[out] if wk is None else wk)

    def copy(self, out, in_, eng="dve", rk=None, wk=None):
        if eng == "act":
            fn = lambda e: e.copy(out=out, in_=in_)
        else:
            fn = lambda e: e.tensor_copy(out=out, in_=in_)
        self.op(eng, fn, reads=[in_] if rk is None else rk, writes=[out] if wk is None else wk)

    def memset(self, ap, val, eng="dve"):
        self.op(eng, lambda e: e.memset(ap, val), reads=[], writes=[ap])

    def recip(self, out, in_):
        self.op("dve", lambda e: e.reciprocal(out=out, in_=in_), reads=[in_], writes=[out])

    def finish(self):
        nc = self.nc
        fin = self._all_tokens()
        engmap = {"pe": "tensor", "dve": "vector", "act": "scalar", "pool": "gpsimd", "sp": "sync"}
        with nc.Block() as block:
            for e in ENG:
                ops = self.ops[e]
                last = fin if e == "sp" else []
                if not ops and not last:
                    continue

                def body(eng, ops=ops, last=last):
                    for waits, fn, (s, n) in ops:
                        for ws, wv in waits:
                            eng.wait_ge(self.sems[ws], wv)
                        fn(eng).then_inc(self.sems[s], n)
                    for ws, wv in last:
                        eng.wait_ge(self.sems[ws], wv)

                getattr(block, engmap[e])(body)
        self.es.close()
        return nc


import numpy as np, time, sys

T = 2048
D = 1024
EPS = 1e-6
GN_EPS = 64e-5
NEG = -30000.0
SLOPES = [2.0 ** (-(h + 1)) for h in range(8)]


def host_consts():
    c = {}
    c["ones_f"] = np.ones((128, 128), np.float32)
    blk = np.zeros((128, 128), np.float32)
    blk[:64, :64] = 1
    blk[64:, 64:] = 1
    c["blk_f"] = blk
    c["ident_f"] = np.eye(128, dtype=np.float32)
    t = np.arange(T)
    c["rmask"] = np.broadcast_to((t % 64 != 0).astype(np.float32)[None, :], (128, T)).copy()
    r = np.arange(64)[:, None]
    col = np.arange(64)[None, :]
    lo = (col < r).astype(np.float32)
    up = (r < col).astype(np.float32)
    upi = (r <= col).astype(np.float32)
    m2 = np.zeros((128, 4, 2, 64), np.float32)
    m2[:, :, 0, :] = np.concatenate([lo, lo], 0)[:, None, :]
    m2[:, :, 1, :] = np.concatenate([up, up], 0)[:, None, :]
    c["rwm2"] = m2.reshape(128, 512)
    m3 = np.zeros((128, 2, 3, 64), np.float32)
    m3[:, :, 0, :] = np.concatenate([up, up], 0)[:, None, :]
    m3[:, :, 1, :] = np.concatenate([upi, upi], 0)[:, None, :]
    m3[:, :, 2, :] = np.concatenate([upi, upi], 0)[:, None, :]
    c["rwm3"] = m3.reshape(128, 384)
    idb = np.zeros((128, 4, 64), np.float32)
    idb[:] = np.concatenate([np.eye(64), np.eye(64)], 0).astype(np.float32)[:, None, :]
    c["identbc"] = idb.reshape(128, 256)
    aq = np.zeros((8, 2, T), np.float32)
    tl = t % 512
    for h in range(8):
        aq[h, 0] = -SLOPES[h] * (tl % 256)
        aq[h, 1] = -SLOPES[h] * (tl - tl % 256)
    c["alibi_q"] = aq.reshape(16, T)
    sl = np.arange(128)[:, None]
    tl5 = np.arange(512)[None, :]
    cneg = np.zeros((128, 4, 512), np.float32)
    wneg = np.zeros((128, 4, 512), np.float32)
    for o in range(4):
        cneg[:, o, :] = np.where(tl5 >= 128 * o + sl, 0.0, NEG)
        wneg[:, o, :] = np.where(tl5 < sl + 128 * o, 0.0, NEG)
    c["cneg"] = cneg.reshape(128, 2048)
    c["wneg"] = wneg.reshape(128, 2048)
    n = np.arange(128)[:, None]
    c["cmpneg"] = np.where(t[None, :] >= 16 * n + 31, 0.0, NEG).astype(np.float32)
    bsw = np.zeros((128, 8, 16), np.float32)
    for h in range(8):
        for idx in range(16):
            bsw[:, h, idx] = SLOPES[h] * (np.arange(128) - 128 * (idx - 3))
    c["bias_sw"] = bsw.reshape(128, 128)
    bc = np.zeros((128, 8, 4), np.float32)
    for h in range(8):
        for tt in range(4):
            bc[:, h, tt] = SLOPES[h] * (16 * np.arange(128) + 31 - 512 * tt)
    c["bias_cmp"] = bc.reshape(128, 32)
    j = np.arange(32)[None, :]
    ov = ((16 * n < 64 * j + 64) & (16 * n + 32 > 64 * j)).astype(np.float32)
    c["ov"] = ov
    ta = np.zeros((128, 8, 32), np.float32)
    tb = np.zeros((128, 8, 32), np.float32)
    for mi, m in enumerate(range(8, 16)):
        tt_ = 128 * m + np.arange(128)
        cur = (tt_ // 64)[:, None]
        forced = (j == 0) | (j == cur) | (j == cur - 1)
        fut = j > cur
        ta[:, mi, :] = 1.0 - forced - fut
        tb[:, mi, :] = 1e4 * forced - 1.0 * fut
    c["topA"] = ta.reshape(128, 256)
    c["topB"] = tb.reshape(128, 256)
    ee = np.zeros((32, 16, 128), np.float32)
    for kt in range(16):
        for s in range(128):
            ee[2 * kt + s // 64, kt, s] = 1.0
    c["eexp"] = ee.reshape(32, 2048)
    oh = np.zeros((8, 8, 128), np.float32)
    for e in range(8):
        oh[e, e, :] = 1.0
    c["onehot8"] = oh.reshape(8, 1024)
    return c


class Ctx:
    stop = 99


class StopBuild(Exception):
    pass


def chk(X, n):
    if X.stop == n:
        raise StopBuild()


def col_load(P, dst, vec, q="sp"):
    P.dma(q, dst, vec.rearrange("(c p o) -> p c o", p=128, o=1), allow_slow_non_contiguous=True)


def load_consts(P, cin):
    C = {}
    for k in ("ones_f", "blk_f", "ident_f"):
        C[k] = P.alloc(k, [128, 128], F32)
        P.dma("sp", C[k][:], cin[k])
    C["ident_b"] = P.alloc("ident_b", [128, 128], BF16)
    P.copy(C["ident_b"][:], C["ident_f"][:])
    C["ones_b"] = P.alloc("ones_b", [128, 128], BF16)
    P.copy(C["ones_b"][:], C["ones_f"][:])
    C["eps"] = P.alloc("eps", [128, 1], F32)
    P.memset(C["eps"][:], EPS)
    C["gneps"] = P.alloc("gneps", [128, 1], F32)
    P.memset(C["gneps"][:], GN_EPS)
    C["one"] = P.alloc("one", [128, 1], F32)
    P.memset(C["one"][:], 1.0)
    C["mhalf"] = P.alloc("mhalf", [128, 1], F32)
    P.memset(C["mhalf"][:], -0.5)
    return C


def rmsnorm_T(P, C, src, g, hT, Tn, h32cb=None):
    m = P.mark()
    W = min(512, Tn)
    gcol = P.alloc("gcol", [128, 8, 1], F32)
    col_load(P, gcol[:], g)
    xts = [P.alloc("xt%d" % i, [128, 8, W], F32) for i in range(2)]
    sqs = [P.alloc("sq%d" % i, [128, 8, W], F32) for i in range(2)]
    rs = [P.alloc("rs%d" % i, [128, W], F32) for i in range(2)]
    for tt in range(Tn // W):
        xt = xts[tt % 2]; sq = sqs[tt % 2]; r = rs[tt % 2]
        P.dma("sp", xt[:], src[:, tt * W:(tt + 1) * W].rearrange("(c p) t -> p c t", p=128))
        P.act(sq[:], xt[:], AF.Square)
        ps = P.psum()
        for dc in range(8):
            P.mm(ps[:, :W], C["ones_f"][:], sq[:, dc, :], start=(dc == 0), stop=(dc == 7))
        P.act(r[:], ps[:, :W], AF.Sqrt, bias=C["eps"][:], scale=1.0 / D)
        P.recip(r[:], r[:])
        for dc in range(8):
            if h32cb is None:
                P.stt(hT[:, dc, tt * W:(tt + 1) * W], xt[:, dc, :], gcol[:, dc, :], r[:], ALU.mult, ALU.mult)
            else:
                h32cb(tt, dc, xt, gcol, r)
    P.release(m)


def proj_psum(P, X, wsb, n, tt):
    ps = P.psum()
    for dc in range(8):
        P.mm(ps[:n, :], wsb[:, dc, :n], X.hT[:, dc, tt * 512:(tt + 1) * 512], start=(dc == 0), stop=(dc == 7))
    return ps


def load_w(P, wsb, w, c0, n, kc=8):
    P.dma("pool", wsb[:, :, :n], w[:, c0:c0 + n].rearrange("(c p) n -> p c n", p=128))


def proj_shift(P, X, c0, n, dst, raw, wsbs, mucols):
    X.psi = getattr(X, "psi", 0) + 1
    wsb = wsbs[X.psi % len(wsbs)]
    mucol = mucols[X.psi % len(mucols)]
    load_w(P, wsb, X.w_in, c0, n)
    P.dma("sp", mucol[:n, :], X.mu[c0:c0 + n].rearrange("(p o) -> p o", o=1), allow_slow_non_contiguous=True)
    for tt in range(4):
        ps = proj_psum(P, X, wsb, n, tt)
        P.copy(raw[:n, 1 + tt * 512:1 + (tt + 1) * 512], ps[:n, :], eng="act")
    P.tt(dst[:n, :], raw[:n, 0:T], raw[:n, 1:T + 1], ALU.subtract)
    P.stt(dst[:n, :], dst[:n, :], mucol[:n, :], raw[:n, 1:T + 1], ALU.mult, ALU.add)


def rwkv_phase(P, C, X, L):
    cin = X.cin
    m0 = P.mark()
    rwm2 = P.alloc("rwm2", [128, 512], BF16)
    P.dma("pool", rwm2[:], cin["rwm2"])
    rwm3 = P.alloc("rwm3", [128, 384], BF16)
    P.dma("pool", rwm3[:], cin["rwm3"])
    identbc = P.alloc("identbc", [128, 256], BF16)
    P.dma("pool", identbc[:], cin["identbc"])
    rmask = P.alloc("rmask", [128, T], F32)
    P.dma("sp", rmask[:], cin["rmask"])
    cols = {}
    for nm in ("rw_w0", "rw_a0", "rw_k_k", "rw_k_a", "rw_ln_w", "rw_ln_b"):
        cols[nm] = P.alloc(nm, [128, 4, 1], F32)
        col_load(P, cols[nm][:], X.inp[nm][L])
    cols["rk"] = P.alloc("rk", [128, 4, 1], F32)
    col_load(P, cols["rk"][:], X.inp["rw_r_k"][L].rearrange("h d -> (h d)"))
    if L > 0:
        cols["v0"] = P.alloc("v0", [128, 4, 1], F32)
        col_load(P, cols["v0"][:], X.inp["rw_v0"][L - 1])
    negw0 = P.alloc("negw0", [128, 4, 1], F32)
    P.ts(negw0[:], cols["rw_w0"][:], -1.0)
    omka = P.alloc("omka", [128, 4, 1], F32)
    P.ts(omka[:], cols["rw_k_a"][:], -1.0, 1.0, ALU.mult, ALU.add)
    wa2 = P.alloc("wa2", [128, 512], BF16)
    P.dma("pool", wa2[0:64, :], X.inp["rw_w2"][L])
    P.dma("pool", wa2[64:128, :], X.inp["rw_a2"][L])
    g2 = P.alloc("g2", [128, 512], BF16)
    P.dma("pool", g2[:], X.inp["rw_g2"][L])
    if L > 0:
        v2 = P.alloc("v2", [32, 512], BF16)
        P.dma("pool", v2[:], X.inp["rw_v2"][L - 1])
    lw_a = P.alloc("lw_a", [128, T], BF16)
    sglo = P.alloc("sglo", [128, T], BF16)
    vlo = P.alloc("vlo", [32, T], BF16) if L > 0 else None
    wsb = [P.alloc("wsb%d" % i, [128, 8, 128], BF16) for i in range(3)]
    mucol = [P.alloc("mucol%d" % i, [128, 1], F32) for i in range(3)]
    raw = P.alloc("raw", [128, T + 1], F32)
    P.memset(raw[:, 0:1], 0.0)
    m1 = P.mark()
    tmp = P.alloc("tmp", [128, T], F32)
    proj_shift(P, X, 1536, 128, tmp, raw, wsb, mucol)
    P.act(lw_a[0:64, :], tmp[0:64, :], AF.Tanh)
    P.copy(lw_a[64:128, :], tmp[64:128, :], eng="act")
    proj_shift(P, X, 1664, 128, tmp, raw, wsb, mucol)
    P.act(sglo[:], tmp[:], AF.Sigmoid)
    if L > 0:
        proj_shift(P, X, 1792, 32, tmp, raw, wsb, mucol)
        P.copy(vlo[:], tmp[0:32, :], eng="act")
    P.release(m1)
    chk(X, 1)

    for ct in range(4):
        mc = P.mark()
        cs = slice(ct * 128, (ct + 1) * 128)
        gT = P.alloc("gT", [128, T], F32)
        bonus = P.alloc("bonus", [128, T], F32)
        mA = P.mark()
        Rt = P.alloc("Rt", [128, T], BF16)
        Kt = P.alloc("Kt", [128, T], BF16)
        Bt = P.alloc("Bt", [128, T], BF16)
        At = P.alloc("At", [128, T], BF16)
        Kh = P.alloc("Kh", [128, T], BF16)
        Bh = P.alloc("Bh", [128, T], BF16)
        Vb = P.alloc("Vb", [128, T], BF16)
        gam = P.alloc("gam", [128, 32], F32)
        mp = P.mark()
        r = P.alloc("r", [128, T], F32)
        k = P.alloc("k", [128, T], F32)
        v = P.alloc("v", [128, T], F32)
        a = P.alloc("a", [128, T], F32)
        e2 = P.alloc("e2", [128, T], F32)
        csn = P.alloc("csn", [128, T], F32)
        kkn = P.alloc("kkn", [128, T], F32)
        b = P.alloc("b", [128, T], F32)
        t1 = P.alloc("t1", [128, T], F32)
        proj_shift(P, X, ct * 128, 128, r, raw, wsb, mucol)
        proj_shift(P, X, 512 + ct * 128, 128, k, raw, wsb, mucol)
        proj_shift(P, X, 1024 + ct * 128, 128, v, raw, wsb, mucol)
        for tt in range(4):
            ts_ = slice(tt * 512, (tt + 1) * 512)
            ps = P.psum()
            P.mm(ps[:, :], wa2[0:64, cs], lw_a[0:64, ts_])
            P.act(t1[:, ts_], ps[:, :], AF.Exp, bias=negw0[:, ct, :], scale=-1.0)
            ps = P.psum()
            P.mm(ps[:, :], wa2[64:128, cs], lw_a[64:128, ts_])
            P.act(a[:, ts_], ps[:, :], AF.Sigmoid, bias=cols["rw_a0"][:, ct, :])
            ps = P.psum()
            P.mm(ps[:, :], g2[:, cs], sglo[:, ts_])
            P.copy(gT[:, ts_], ps[:, :], eng="act")
        P.act(t1[:], t1[:], AF.Ln, bias=C["one"][:])
        P.act(e2[:], t1[:], AF.Exp, bias=C["mhalf"][:], scale=-1.0)
        if L > 0:
            vf = b
            P.dma("sp", vf[:], X.vfirst[cs, :])
            for tt in range(4):
                ts_ = slice(tt * 512, (tt + 1) * 512)
                ps = P.psum()
                P.mm(ps[:, :], v2[0:32, cs], vlo[0:32, ts_])
                P.act(t1[:, ts_], ps[:, :], AF.Sigmoid, bias=cols["v0"][:, ct, :])
            P.tt(vf[:], vf[:], v[:], ALU.subtract)
            P.tt(vf[:], vf[:], t1[:], ALU.mult)
            P.tt(v[:], v[:], vf[:], ALU.add)
        else:
            P.dma("sp", X.vfirst[cs, :], v[:])
        P.ts(kkn[:], k[:], cols["rw_k_k"][:, ct, :])
        P.tt(t1[:], kkn[:], kkn[:], ALU.mult)
        for tt in range(4):
            ts_ = slice(tt * 512, (tt + 1) * 512)
            ps = P.psum()
            P.mm(ps[:, :], C["blk_f"][:], t1[:, ts_])
            P.act(b[:, ts_], ps[:, :], AF.Sqrt)
        P.ts(b[:], b[:], 1e-12, None, ALU.max)
        P.recip(b[:], b[:])
        P.tt(kkn[:], kkn[:], b[:], ALU.mult)
        P.ts(t1[:], a[:], cols["rw_k_a"][:, ct, :], omka[:, ct, :], ALU.mult, ALU.add)
        P.tt(k[:], k[:], t1[:], ALU.mult)
        P.tt(b[:], kkn[:], a[:], ALU.mult)
        P.stt(t1[:], r[:], cols["rk"][:, ct, :], k[:], ALU.mult, ALU.mult)
        for tt in range(4):
            ts_ = slice(tt * 512, (tt + 1) * 512)
            ps = P.psum()
            P.mm(ps[:, :], C["blk_f"][:], t1[:, ts_])
            P.tt(bonus[:, ts_], ps[:, :], v[:, ts_], ALU.mult)
        P.copy(Vb[:], v[:], eng="act")
        P.op("dve", lambda e: e.tensor_tensor_scan(out=csn[:], data0=rmask[:], data1=e2[:], initial=0.0,
                                                   op0=ALU.mult, op1=ALU.add), reads=[rmask, e2], writes=[csn])
        P.act(t1[:], csn[:], AF.Exp, scale=-1.0)
        P.tt(Rt[:], r[:], t1[:], ALU.mult)
        P.act(t1[:], csn[:], AF.Exp)
        P.tt(Kt[:], k[:], t1[:], ALU.mult)
        P.tt(Bt[:], b[:], t1[:], ALU.mult)
        P.tt(t1[:], csn[:], e2[:], ALU.subtract)
        P.act(t1[:], t1[:], AF.Exp, scale=-1.0)
        P.stt(At[:], kkn[:], -1.0, t1[:], ALU.mult, ALU.mult)
        totn = csn[:, 63:T:64]
        P.act(gam[:], totn, AF.Exp, scale=-1.0)
        csn3 = csn[:].rearrange("p (c s) -> p c s", s=64)
        t13 = t1[:].rearrange("p (c s) -> p c s", s=64)
        P.tt(t13, totn.unsqueeze(2).to_broadcast([128, 32, 64]), csn3, ALU.subtract)
        P.act(t1[:], t1[:], AF.Exp, scale=-1.0)
        P.tt(Kh[:], k[:], t1[:], ALU.mult)
        P.tt(Bh[:], b[:], t1[:], ALU.mult)
        P.release(mp)
        chk(X, 2)

        ytok = P.alloc("ytok", [128, 32, 64], F32, split=True)
        mq = P.mark()
        Vtok = P.alloc("Vtok", [128, 32, 64], BF16, split=True)
        Khtok = P.alloc("Khtok", [128, 32, 64], BF16, split=True)
        Bhtok = P.alloc("Bhtok", [128, 32, 64], BF16, split=True)
        IM = P.alloc("IM", [128, 32, 192], BF16, split=True)
        TTg = [P.alloc("TT%d" % g, [128, 4, 64], BF16, split=True) for g in range(8)]
        LU = [[P.alloc("LU%d_%d" % (s, pp), [128, 4, 128], BF16, split=True) for pp in range(2)] for s in range(2)]
        PB = [slice(0, 64), slice(64, 128)]
        for src, dst in ((Vb, Vtok), (Kh, Khtok), (Bh, Bhtok)):
            for c8 in range(4):
                for hh in range(2):
                    pb = PB[hh]
                    ps = P.psum()
                    for ci in range(8):
                        c = c8 * 8 + ci
                        P.mm(ps[pb, ci * 64:(ci + 1) * 64], src[pb, c * 64:(c + 1) * 64], C["ident_b"][pb, pb])
                    P.copy(dst[pb, c8 * 8:(c8 + 1) * 8, :].rearrange("p a b -> p (a b)"), ps[pb, :],
                           eng=("act" if hh else "dve"))
        chk(X, 3)
        for c2 in range(16):
            for hh in range(2):
                pb = PB[hh]
                ps = P.psum()
                for ci in range(2):
                    c = c2 * 2 + ci
                    sl = slice(c * 64, (c + 1) * 64)
                    o = ci * 192
                    P.mm(ps[pb, o:o + 64], Kt[pb, sl], At[pb, sl])
                    P.mm(ps[pb, o + 64:o + 128], Bt[pb, sl], Rt[pb, sl])
                    P.mm(ps[pb, o + 128:o + 192], Kt[pb, sl], Rt[pb, sl])
                P.tt(IM[pb, c2 * 2:c2 * 2 + 2, :].rearrange("p a b -> p (a b)"), ps[pb, 0:384], rwm3[pb, :], ALU.mult)
        chk(X, 4)
        for gp in range(4):
            grp = [gp * 2, gp * 2 + 1]
            comb = [(s, g, hh) for s, g in enumerate(grp) for hh in range(2)]
            for s, g, hh in comb:
                pb = PB[hh]
                ps = P.psum()
                for ci in range(4):
                    c = g * 4 + ci
                    sl = slice(c * 64, (c + 1) * 64)
                    o = ci * 128
                    P.mm(ps[pb, o:o + 64], At[pb, sl], Bt[pb, sl])
                    P.mm(ps[pb, o + 64:o + 128], Bt[pb, sl], At[pb, sl])
                P.tt(LU[s][0][pb].rearrange("p a b -> p (a b)"), ps[pb, :], rwm2[pb, :], ALU.mult)
                P.tt(TTg[g][pb], LU[s][0][pb, :, 64:128], identbc[pb].rearrange("p (a b) -> p a b", a=4), ALU.add)
            for lev in range(1, 6):
                pi, po = (lev - 1) % 2, lev % 2
                psl = {}
                for s, g, hh in comb:
                    pb = PB[hh]
                    ps = P.psum()
                    psl[(s, hh)] = ps
                    for ci in range(4):
                        o = ci * 128
                        Lp = LU[s][pi][pb, ci, 0:64]
                        Up = LU[s][pi][pb, ci, 64:128]
                        P.mm(ps[pb, o:o + 64], Up, Lp)
                        P.mm(ps[pb, o + 64:o + 128], Lp, Up)
                    if (s, hh) == (0, 1) or (s, hh) == (1, 1):
                        for hh2 in range(2):
                            P.copy(LU[s][po][PB[hh2]].rearrange("p a b -> p (a b)"), psl[(s, hh2)][PB[hh2], :],
                                   eng=("act" if hh2 else "dve"))
                pst = {}
                for s, g, hh in comb:
                    pb = PB[hh]
                    ps = P.psum()
                    pst[(s, hh)] = ps
                    for ci in range(4):
                        P.mm(ps[pb, ci * 64:(ci + 1) * 64], LU[s][po][pb, ci, 0:64], TTg[g][pb, ci, :])
                for s, g, hh in comb:
                    pb = PB[hh]
                    P.tt(TTg[g][pb].rearrange("p a b -> p (a b)"), TTg[g][pb].rearrange("p a b -> p (a b)"),
                         pst[(s, hh)][pb, 0:256], ALU.add)
        chk(X, 5)
        Pst = P.alloc("Pst", [128, 64], F32, split=True)
        Pbf = P.alloc("Pbf", [128, 64], BF16, split=True)
        P.memset(Pst[:], 0.0)
        P.memset(Pbf[:], 0.0)
        r1s = [P.alloc("r1_%d" % i, [128, 64], BF16, split=True) for i in range(2)]
        Us = [P.alloc("U_%d" % i, [128, 64], BF16, split=True) for i in range(2)]
        for c in range(32):
            sl = slice(c * 64, (c + 1) * 64)
            g, ci = c // 4, c % 4
            r1 = r1s[c % 2]; U = Us[c % 2]
            for hh in range(2):
                pb = PB[hh]
                ps = P.psum()
                P.mm(ps[pb, 0:64], IM[pb, c, 0:64], Vtok[pb, c, :], start=True, stop=False)
                P.mm(ps[pb, 0:64], At[pb, sl], Pbf[pb, :], start=False, stop=True)
                P.copy(r1[pb, :], ps[pb, 0:64], eng=("act" if hh else "dve"))
            for hh in range(2):
                pb = PB[hh]
                ps = P.psum()
                P.mm(ps[pb, 0:64], TTg[g][pb, ci, :], r1[pb, :])
                P.copy(U[pb, :], ps[pb, 0:64], eng=("act" if hh else "dve"))
            for hh in range(2):
                pb = PB[hh]
                ps = P.psum()
                P.mm(ps[pb, 0:64], Bhtok[pb, c, :], U[pb, :], start=True, stop=False)
                P.mm(ps[pb, 0:64], Khtok[pb, c, :], Vtok[pb, c, :], start=False, stop=True)
                psy = P.psum()
                P.mm(psy[pb, 0:64], Rt[pb, sl], Pbf[pb, :], start=True, stop=False)
                P.mm(psy[pb, 0:64], IM[pb, c, 64:128], U[pb, :], start=False, stop=False)
                P.mm(psy[pb, 0:64], IM[pb, c, 128:192], Vtok[pb, c, :], start=False, stop=True)
                P.stt(Pbf[pb, :], Pst[pb, :], gam[pb, c:c + 1], ps[pb, 0:64], ALU.mult, ALU.add)
                P.stt(Pst[pb, :], Pst[pb, :], gam[pb, c:c + 1], ps[pb, 0:64], ALU.mult, ALU.add)
                P.copy(ytok[pb, c, :], psy[pb, 0:64], eng=("act" if hh else "dve"))
        chk(X, 6)
        P.release(mq)
        sq = P.alloc("gsq", [128, 32, 64], F32)
        s1 = P.alloc("gs1", [128, 32], F32)
        s2 = P.alloc("gs2", [128, 32], F32)
        yv = ytok[:]
        P.tt(sq[:], yv, yv, ALU.mult)
        P.op("dve", lambda e: e.tensor_reduce(out=s1[:], in_=yv, axis=AX.X, op=ALU.add), reads=[ytok], writes=[s1])
        P.op("dve", lambda e: e.tensor_reduce(out=s2[:], in_=sq[:], axis=AX.X, op=ALU.add), reads=[sq], writes=[s2])
        P.ts(s1[:], s1[:], 1.0 / 64)
        P.ts(s2[:], s2[:], 1.0 / 64)
        m2_ = P.alloc("gm2", [128, 32], F32)
        P.tt(m2_[:], s1[:], s1[:], ALU.mult)
        P.tt(s2[:], s2[:], m2_[:], ALU.subtract)
        P.act(s2[:], s2[:], AF.Sqrt, bias=C["gneps"][:])
        P.recip(s2[:], s2[:])
        P.tt(yv, yv, s1[:].unsqueeze(2).to_broadcast([128, 32, 64]), ALU.subtract)
        P.tt(yv, yv, s2[:].unsqueeze(2).to_broadcast([128, 32, 64]), ALU.mult)
        yo = P.alloc("yo", [128, T], F32, split=True)
        for c8 in range(4):
            for hh in range(2):
                pb = PB[hh]
                ps = P.psum()
                for ci in range(8):
                    c = c8 * 8 + ci
                    P.mm(ps[pb, ci * 64:(ci + 1) * 64], ytok[pb, c, :], C["ident_f"][pb, pb])
                P.ts(yo[pb, c8 * 512:(c8 + 1) * 512], ps[pb, :], cols["rw_ln_w"][pb, ct, :], cols["rw_ln_b"][pb, ct, :],
                     ALU.mult, ALU.add)
        P.tt(yo[:], yo[:], bonus[:], ALU.add)
        ob = P.alloc("ob", [128, T], BF16)
        P.tt(ob[:], yo[:], gT[:], ALU.mult)
        P.dma("sp", X.ymix[cs, :], ob[:])
        if X.dbg is not None and "yrw" in X.dbg:
            P.tt(yo[:], yo[:], gT[:], ALU.mult)
            P.dma("sp", X.dbg["yrw"][cs, :], yo[:])
        P.release(mc)
        chk(X, 7)
    P.release(m0)


def nsa_phase(P, C, X, L):
    cin = X.cin
    inp = X.inp
    n_rw = X.n_rw
    m0 = P.mark()
    def ctab(name, shape, dt=BF16, q="pool"):
        b = P.alloc(name, shape, dt)
        P.dma(q, b[:], cin[name])
        return b
    cneg = ctab("cneg", [128, 2048])
    wneg = ctab("wneg", [128, 2048])
    cmpneg = ctab("cmpneg", [128, 2048])
    eexp = P.alloc("eexp", [128, 2048], BF16)
    P.memset(eexp[:], 0.0, eng="dve")
    P.dma("pool", eexp[0:32, :], cin["eexp"])
    bias_sw = ctab("bias_sw", [128, 128], F32, "sp")
    bias_cmp = ctab("bias_cmp", [128, 32], F32, "sp")
    topA = ctab("topA", [128, 256], F32, "sp")
    topB = ctab("topB", [128, 256], F32, "sp")
    qa = [P.alloc("qa%d" % h, [128, T], BF16) for h in range(8)]
    ka = {}
    for br in (1, 2):
        for g in range(2):
            ka[(br, g)] = P.alloc("ka%d%d" % (br, g), [128, T], BF16)
    vtok = {}
    for br in (1, 2):
        for g in range(2):
            vtok[(br, g)] = P.alloc("vtok%d%d" % (br, g), [128, 16, 66], BF16)
    kc_a = [P.alloc("kc_a%d" % g, [128, 128], BF16) for g in range(2)]
    vc_aug = [P.alloc("vc_aug%d" % g, [128, 98], BF16) for g in range(2)]
    gates_tok = P.alloc("gates_tok", [128, 16, 24], F32)
    ynsa = P.alloc("ynsa", [128, 16, 512], F32)
    imp = P.alloc("imp", [128, 8, 2, 32], F32)
    negselT = [P.alloc("negselT%d" % g, [128, T], BF16) for g in range(2)]
    for h in range(8):
        P.memset(qa[h][:], 0.0, eng="dve")
        P.dma("pool", qa[h][64:66, :], cin["alibi_q"][2 * h:2 * h + 2, :])
    for kk_ in ka.values():
        P.memset(kk_[:], 0.0, eng="dve")
        P.memset(kk_[64:66, :], 1.0, eng="dve")
    for g in range(2):
        P.memset(kc_a[g][:], 0.0, eng="dve")
        P.memset(kc_a[g][64:66, :], 1.0, eng="dve")
        P.memset(vc_aug[g][:], 0.0, eng="dve")
        P.memset(vc_aug[g][:, 64:65], 1.0, eng="dve")
        P.dma("pool", vc_aug[g][:, 65:97], cin["ov"])
        P.memset(negselT[g][:], 0.0, eng="dve")
    for vt in vtok.values():
        P.memset(vt[:, :, 64:65], 1.0, eng="dve")
    wsbl = [P.alloc("wsbn%d" % i, [128, 8, 64], BF16) for i in range(3)]
    wsi = [0]

    def proj_piece(c0, n, epi):
        wsb = wsbl[wsi[0] % 3]
        wsi[0] += 1
        load_w(P, wsb, X.w_in, n_rw + c0, n)
        for tt in range(4):
            ps = proj_psum(P, X, wsb, n, tt)
            epi(tt, ps)

    for h in range(8):
        proj_piece(h * 64, 64, lambda tt, ps, h=h: P.act(qa[h][0:64, tt * 512:(tt + 1) * 512], ps[0:64, :], AF.Copy, scale=0.125))
    for br in (1, 2):
        for g in range(2):
            c0 = 512 + (2 * br) * 128 + g * 64
            proj_piece(c0, 64, lambda tt, ps, br=br, g=g: P.copy(ka[(br, g)][0:64, tt * 512:(tt + 1) * 512], ps[0:64, :], eng="act"))
    mt = P.mark()
    vT = P.alloc("vT", [64, T], BF16)
    for br in (1, 2):
        for g in range(2):
            c0 = 512 + (2 * br + 1) * 128 + g * 64
            proj_piece(c0, 64, lambda tt, ps: P.copy(vT[0:64, tt * 512:(tt + 1) * 512], ps[0:64, :], eng="act"))
            for t8 in range(2):
                ps = P.psum()
                for ti in range(8):
                    tile = t8 * 8 + ti
                    P.mm(ps[:, ti * 64:(ti + 1) * 64], vT[0:64, tile * 128:(tile + 1) * 128], C["ident_b"][0:64, 0:64])
                P.copy(vtok[(br, g)][:, t8 * 8:(t8 + 1) * 8, 0:64], ps[:, :].rearrange("p (a b) -> p a b", a=8), eng="dve")
    gsT = P.alloc("gsT", [24, T], F32)
    proj_piece(1280, 24, lambda tt, ps: P.act(gsT[0:24, tt * 512:(tt + 1) * 512], ps[0:24, :], AF.Sigmoid))
    ps = P.psum()
    for tile in range(16):
        P.mm(ps[:, tile * 24:(tile + 1) * 24], gsT[0:24, tile * 128:(tile + 1) * 128], C["ident_f"][0:24, 0:24])
    P.copy(gates_tok[:].rearrange("p a b -> p (a b)"), ps[:, 0:384], eng="dve")
    kcT = [P.alloc("kcT%d" % g, [64, T], BF16) for g in range(2)]
    w1 = P.alloc("w1", [64, 32, 128], BF16)
    peT = P.alloc("peT", [64, 32, 2], BF16)
    w2 = P.alloc("w2", [128, 64], BF16)
    bvec = P.alloc("bvec", [128, 1], F32)
    xs = P.alloc("xs", [128, 128], F32)
    x2 = P.alloc("x2", [128, 128], F32)
    gl = P.alloc("gl", [128, 128], BF16)
    for kind in range(2):
        nm = "cmp_k" if kind == 0 else "cmp_v"
        for g in range(2):
            c0 = 512 + kind * 128 + g * 64
            proj_piece(c0, 64, lambda tt, ps, g=g: P.copy(kcT[g][0:64, tt * 512:(tt + 1) * 512], ps[0:64, :], eng="act"))
        P.dma("pool", w1[:], inp[nm + "_w1"][L].rearrange("(l d) h -> d l h", d=64))
        for j in range(2):
            P.dma("pool", peT[:, :, j:j + 1], inp[nm + "_pe"][L].rearrange("l (d o) -> d l o", o=1), allow_slow_non_contiguous=True)
        P.dma("pool", w2[:], inp[nm + "_w2"][L])
        ps = P.psum()
        for l in range(32):
            P.mm(ps[:, 0:2], w1[:, l, :], peT[:, l, :], start=(l == 0), stop=(l == 31))
        P.copy(bvec[:], ps[:, 0:1], eng="act")
        for g in range(2):
            ps = P.psum()
            for l in range(32):
                P.mm(ps[:, 0:127], w1[:, l, :], kcT[g][0:64, l:l + 16 * 126 + 1:16], start=(l == 0), stop=(l == 31))
            P.act(xs[:, 0:127], ps[:, 0:127], AF.Identity, bias=bvec[:])
            P.tt(x2[:, 0:127], xs[:, 0:127], xs[:, 0:127], ALU.mult)
            P.ts(x2[:, 0:127], x2[:, 0:127], 0.044715, 1.0, ALU.mult, ALU.add)
            P.tt(x2[:, 0:127], x2[:, 0:127], xs[:, 0:127], ALU.mult)
            P.act(x2[:, 0:127], x2[:, 0:127], AF.Sigmoid, scale=1.5957691216057308)
            P.tt(gl[:, 0:127], xs[:, 0:127], x2[:, 0:127], ALU.mult)
            ps = P.psum()
            if kind == 0:
                P.mm(ps[0:64, 0:127], w2[:, :], gl[:, 0:127])
                P.copy(kc_a[g][0:64, 0:127], ps[0:64, 0:127], eng="act")
            else:
                P.mm(ps[0:127, 0:64], gl[:, 0:127], w2[:, :])
                P.copy(vc_aug[g][0:127, 0:64], ps[0:127, 0:64], eng="act")
    P.release(mt)
    chk(X, 11)

    eTs = [P.alloc("eT%d" % i, [128, 512], BF16) for i in range(4)]
    eti = [0]
    rsb = P.alloc("rsb", [128, 8], F32)
    rsi = [0]

    def next_eT():
        e = eTs[eti[0] % 4]
        eti[0] += 1
        return e

    def epilogue(acc, m, h, br, first):
        i = rsi[0] % 4
        rsi[0] += 1
        rs = rsb[:, 2 * i:2 * i + 1]
        cf = rsb[:, 2 * i + 1:2 * i + 2]
        P.ts(rs, acc[:, 64:65], 1e-30, None, ALU.max)
        P.recip(rs, rs)
        P.tt(cf, rs, gates_tok[:, m, h * 3 + br:h * 3 + br + 1], ALU.mult)
        dst = ynsa[:, m, h * 64:(h + 1) * 64]
        if first:
            P.ts(dst, acc[:, 0:64], cf, None, ALU.mult)
        else:
            P.stt(dst, acc[:, 0:64], cf, dst, ALU.mult, ALU.add)
        return rs

    rs4 = P.alloc("rs4", [128, 2, 4], F32)
    cf4 = P.alloc("cf4", [128, 2, 4], F32)
    ei = 0
    for h in range(8):
        g, r = h // 4, h % 4
        for tt in range(4):
            ts_ = slice(tt * 512, (tt + 1) * 512)
            ps = P.psum()
            P.mm(ps[0:127, :], kc_a[g][:, 0:127], qa[h][:, ts_], start=True, stop=False)
            P.mm(ps[0:127, :], C["ident_b"][:, 0:127], cmpneg[:, ts_], start=False, stop=True)
            eT = next_eT()
            P.act(eT[0:127, :], ps[0:127, :], AF.Exp, bias=bias_cmp[0:127, h * 4 + tt:h * 4 + tt + 1])
            acc = P.psum()
            for sub in range(4):
                P.mm(acc[:, sub * 97:(sub + 1) * 97], eT[0:127, sub * 128:(sub + 1) * 128], vc_aug[g][0:127, 0:97])
            av = acc[:, 0:388].rearrange("p (a b) -> p a b", a=4)
            rs = rs4[:, ei % 2, :]
            cf = cf4[:, ei % 2, :]
            ei += 1
            P.ts(rs, av[:, :, 64], 1e-30, None, ALU.max)
            P.recip(rs, rs)
            P.tt(cf, rs, gates_tok[:, 4 * tt:4 * tt + 4, h * 3], ALU.mult)
            P.tt(ynsa[:, 4 * tt:4 * tt + 4, h * 64:(h + 1) * 64], av[:, :, 0:64],
                 cf.unsqueeze(2).to_broadcast([128, 4, 64]), ALU.mult)
            if tt >= 2:
                iv = imp[:, 4 * (tt - 2):4 * (tt - 2) + 4, g, :]
                if r == 0:
                    P.tt(iv, av[:, :, 65:97], rs.unsqueeze(2).to_broadcast([128, 4, 32]), ALU.mult)
                else:
                    tmp4 = P.alloc("imptmp%d_%d" % (h, tt), [128, 4, 32], F32)
                    P.tt(tmp4[:], av[:, :, 65:97], rs.unsqueeze(2).to_broadcast([128, 4, 32]), ALU.mult)
                    P.tt(iv, iv, tmp4[:], ALU.add)
    chk(X, 12)
    mx = P.alloc("mx", [128, 16], F32)
    wk_ = P.alloc("wk", [128, 32], F32)
    sel = P.alloc("sel", [128, 32], F32)
    nsb = P.alloc("nsb", [128, 32], BF16)
    for mi in range(8):
        for g in range(2):
            iv = imp[:, mi, g, :]
            P.tt(iv, iv, topA[:, mi * 32:(mi + 1) * 32], ALU.mult)
            P.tt(iv, iv, topB[:, mi * 32:(mi + 1) * 32], ALU.add)
            P.op("dve", lambda e, iv=iv: e.max(out=mx[:, 0:8], in_=iv), reads=[imp], writes=[mx])
            P.op("dve", lambda e, iv=iv: e.match_replace(out=wk_[:], in_to_replace=mx[:, 0:8], in_values=iv, imm_value=-2.0),
                 reads=[imp, mx], writes=[wk_])
            P.op("dve", lambda e: e.max(out=mx[:, 8:16], in_=wk_[:]), reads=[wk_], writes=[mx])
            P.ts(sel[:], iv, mx[:, 15:16], None, ALU.is_ge)
            P.ts(nsb[:], sel[:], -NEG, NEG, ALU.mult, ALU.add)
            ps = P.psum()
            P.mm(ps[0:32, 0:128], nsb[:, :], C["ident_b"][:, :])
            P.copy(negselT[g][0:32, (8 + mi) * 128:(9 + mi) * 128], ps[0:32, 0:128], eng="act")
    chk(X, 13)
    sb_i = [0]
    accs = [P.banks[i] for i in range(4)]
    jobs = []
    for h in range(8):
        for tt in range(4):
            for br in (1, 2):
                kt0 = 0 if br == 1 else max(0, 4 * tt - 4)
                for kt in range(kt0, 4 * tt + 4):
                    jobs.append((h, tt, br, kt, kt == 4 * tt + 3))

    def emit_score(job):
        h, tt, br, kt, last = job
        g = h // 4
        t0 = tt * 512
        ps = P.banks[4 + sb_i[0] % 4]
        sb_i[0] += 1
        c0, c1, mk = 0, 512, None
        if kt >= 4 * tt:
            o = kt - 4 * tt
            c0 = 128 * o
            mk = (cneg[:, 0:128], 128 * o)
        elif br == 2:
            o = kt - (4 * tt - 4)
            c1 = 128 * (o + 1)
            mk = (wneg[:, 0:128], 128 * o)
        mms = [(ka[(br, g)][:, kt * 128:(kt + 1) * 128], qa[h][:, t0 + c0:t0 + c1], c0, c1)]
        if br == 1 and tt >= 2:
            mms.append((eexp[:, kt * 128:(kt + 1) * 128], negselT[g][:, t0 + c0:t0 + c1], c0, c1))
        if mk is not None:
            mms.append((C["ident_b"][:, :], mk[0], mk[1], mk[1] + 128))
        for i, (l_, r_, a0, a1) in enumerate(mms):
            P.mm(ps[:, a0:a1], l_, r_, start=(i == 0), stop=(i == len(mms) - 1))
        eT = next_eT()
        bidx = h * 16 + (4 * tt - kt + 3)
        P.act(eT[:, c0:c1], ps[:, c0:c1], AF.Exp, bias=bias_sw[:, bidx:bidx + 1])
        return eT

    def emit_pv(job, eT):
        h, tt, br, kt, last = job
        g = h // 4
        for sub in range(4):
            hi = 4 * tt + sub
            lo = 0 if br == 1 else max(0, hi - 4)
            if lo <= kt <= hi:
                P.mm(accs[sub][:, 0:65], eT[:, sub * 128:(sub + 1) * 128], vtok[(br, g)][:, kt, 0:65],
                     start=(kt == lo), stop=(kt == hi))
        if last:
            for sub in range(4):
                epilogue(accs[sub], 4 * tt + sub, h, br, False)

    pend = []
    for job in jobs:
        eT = emit_score(job)
        pend.append((job, eT))
        if len(pend) > 2:
            emit_pv(*pend.pop(0))
    while pend:
        emit_pv(*pend.pop(0))
    chk(X, 14)
    stg = [P.alloc("nstg%d" % i, [128, 512], BF16) for i in range(2)]
    si = 0
    for cg in range(4):
        for m4 in range(4):
            ps = P.psum()
            for j in range(4):
                m = m4 * 4 + j
                P.mm(ps[:, j * 128:(j + 1) * 128], ynsa[:, m, cg * 128:(cg + 1) * 128], C["ident_f"][:, :])
            s = stg[si % 2]
            si += 1
            P.copy(s[:], ps[:, :], eng="act")
            P.dma("sp", X.ymix[512 + cg * 128:512 + (cg + 1) * 128, m4 * 512:(m4 + 1) * 512], s[:], wk=[("ymixn", cg, m4)])
            if X.dbg is not None and "ynsa" in X.dbg:
                if not hasattr(X, "_dbg32"):
                    X._dbg32 = [P.alloc("dbgs%d" % i, [128, 512], F32) for i in range(2)]
                s32 = X._dbg32[si % 2]
                P.copy(s32[:], ps[:, :], eng="dve")
                P.dma("sp", X.dbg["ynsa"][cg * 128:(cg + 1) * 128, m4 * 512:(m4 + 1) * 512], s32[:])
    P.release(m0)


def resid_proj(P, C, X, actT, W, KC, x_src, x_dst):
    m = P.mark()
    wo = P.alloc("wo", [128, KC, 1024], BF16)
    for q4 in range(4):
        P.dma("pool", wo[:, :, q4 * 256:(q4 + 1) * 256], W[:, q4 * 256:(q4 + 1) * 256].rearrange("(c p) n -> p c n", p=128))
    xts = [P.alloc("rxt%d" % i, [128, 512], F32) for i in range(4)]
    i = 0
    for dc in range(8):
        for tt in range(4):
            ts_ = slice(tt * 512, (tt + 1) * 512)
            xt = xts[i % 4]
            i += 1
            P.dma("sp", xt[:], x_src[dc * 128:(dc + 1) * 128, ts_], rk=[("xsrc", dc, tt)])
            ps = P.psum()
            for kc in range(KC):
                P.mm(ps[:, :], wo[:, kc, dc * 128:(dc + 1) * 128], actT[:, kc, ts_], start=(kc == 0), stop=(kc == KC - 1))
            P.tt(xt[:], xt[:], ps[:, :], ALU.add)
            P.dma("sp", x_dst[dc * 128:(dc + 1) * 128, ts_], xt[:], wk=[("xsrc", dc, tt)])
    P.release(m)


def wout_phase(P, C, X, L, x_src, x_dst):
    m = P.mark()
    ym = P.alloc("ym", [128, 8, T], BF16)
    for q4 in range(4):
        P.dma("sp", ym[:, q4 * 2:q4 * 2 + 2, :], X.ymix[q4 * 256:(q4 + 1) * 256, :].rearrange("(c p) t -> p c t", p=128))
    resid_proj(P, C, X, ym, X.inp["w_out"][L], 8, x_src, x_dst)
    P.release(m)


def cross_phase(P, C, X, L, x_src, x_dst):
    inp = X.inp
    m0 = P.mark()
    hT = P.alloc("hTc", [128, 8, T], BF16)
    rmsnorm_T(P, C, x_src, inp["cross_norm_g"][L], hT, T)
    mTn = P.alloc("mTn", [128, 8, 256], BF16)
    rmsnorm_T(P, C, X.memT, inp["mem_norm_g"][L], mTn, 256)
    KT = P.alloc("KT", [128, 8, 256], BF16)
    Vtok = P.alloc("Vtokc", [128, 2, 1024], BF16)
    qT = P.alloc("qT", [128, 8, T], BF16)
    oT = P.alloc("oT", [128, 8, T], BF16)
    wkv = inp["cross_wkv"][L]
    wq = inp["cross_wq"][L]
    m1 = P.mark()
    wsbs = [P.alloc("wsbc%d" % i, [128, 8, 128], BF16) for i in range(2)]
    wv = P.alloc("wv", [128, 8, 1024], BF16)
    for q4 in range(4):
        P.dma("pool", wv[:, :, q4 * 256:(q4 + 1) * 256],
              wkv[:, 1024 + q4 * 256:1024 + (q4 + 1) * 256].rearrange("(c p) n -> p c n", p=128))
    for cg in range(8):
        wsb = wsbs[cg % 2]
        load_w(P, wsb, wkv, cg * 128, 128)
        ps = P.psum()
        for dc in range(8):
            P.mm(ps[:, 0:256], wsb[:, dc, :], mTn[:, dc, :], start=(dc == 0), stop=(dc == 7))
        P.copy(KT[:, cg, :], ps[:, 0:256], eng="act")
    for mt in range(2):
        for vg in range(2):
            ps = P.psum()
            for dc in range(8):
                P.mm(ps[:, :], mTn[:, dc, mt * 128:(mt + 1) * 128], wv[:, dc, vg * 512:(vg + 1) * 512],
                     start=(dc == 0), stop=(dc == 7))
            P.copy(Vtok[:, mt, vg * 512:(vg + 1) * 512], ps[:, :], eng="act")
    for cg in range(8):
        wsb = wsbs[cg % 2]
        load_w(P, wsb, wq, cg * 128, 128)
        for tt in range(4):
            ps = P.psum()
            for dc in range(8):
                P.mm(ps[:, :], wsb[:, dc, :], hT[:, dc, tt * 512:(tt + 1) * 512], start=(dc == 0), stop=(dc == 7))
            P.copy(qT[:, cg, tt * 512:(tt + 1) * 512], ps[:, :], eng="act")
    P.release(m1)
    eTs = [P.alloc("eTc%d" % i, [128, 2, 512], BF16) for i in range(2)]
    rsbs = [P.alloc("rsbc%d" % i, [128, 512], F32) for i in range(2)]
    it = 0
    for hd in range(4):
        for tt in range(4):
            ts_ = slice(tt * 512, (tt + 1) * 512)
            eT = eTs[it % 2]; rsb = rsbs[it % 2]
            it += 1
            for mt in range(2):
                ps = P.psum()
                for sub in range(2):
                    P.mm(ps[:, :], KT[:, hd * 2 + sub, mt * 128:(mt + 1) * 128], qT[:, hd * 2 + sub, ts_],
                         start=(sub == 0), stop=(sub == 1))
                P.act(eT[:, mt, :], ps[:, :], AF.Exp, scale=1.0 / 16.0)
            ps = P.psum()
            for mt in range(2):
                P.mm(ps[:, :], C["ones_b"][:, :], eT[:, mt, :], start=(mt == 0), stop=(mt == 1))
            P.recip(rsb[:], ps[:, :])
            for ds in range(2):
                ps = P.psum()
                for mt in range(2):
                    P.mm(ps[:, :], Vtok[:, mt, hd * 256 + ds * 128:hd * 256 + (ds + 1) * 128], eT[:, mt, :],
                         start=(mt == 0), stop=(mt == 1))
                P.tt(oT[:, hd * 2 + ds, ts_], ps[:, :], rsb[:], ALU.mult)
    resid_proj(P, C, X, oT, inp["cross_wo"][L], 8, x_src, x_dst)
    P.release(m0)


def ffn_core(P, C, X, hT, xacc, wg, wu, wd, F, bufs, gbc=None):
    nfc = F // 128
    GS = 4
    wgs, wus, wds, actTs, t1s, t2s = bufs
    gi = 0
    for f0 in range(0, nfc, GS):
        grp = list(range(f0, min(nfc, f0 + GS)))
        actT = actTs[gi % 2]; wdb = wds[gi % 2]
        gi += 1
        P.dma("pool", wdb[:, 0:len(grp), :], wd[f0 * 128:(f0 + len(grp)) * 128, :].rearrange("(c p) n -> p c n", p=128))
        for j, fc in enumerate(grp):
            wgb = wgs[fc % 2]; wub = wus[fc % 2]
            load_w(P, wgb, wg, fc * 128, 128)
            load_w(P, wub, wu, fc * 128, 128)
            for tt in range(4):
                ts_ = slice(tt * 512, (tt + 1) * 512)
                psg = P.psum()
                for dc in range(8):
                    P.mm(psg[:, :], wgb[:, dc, :], hT[:, dc, ts_], start=(dc == 0), stop=(dc == 7))
                psu = P.psum()
                for dc in range(8):
                    P.mm(psu[:, :], wub[:, dc, :], hT[:, dc, ts_], start=(dc == 0), stop=(dc == 7))
                t1 = t1s[tt % 2]
                P.act(t1[:], psg[:, :], AF.Silu)
                if gbc is None:
                    P.tt(actT[:, j, ts_], t1[:], psu[:, :], ALU.mult)
                else:
                    t2 = t2s[tt % 2]
                    P.tt(t2[:], t1[:], psu[:, :], ALU.mult)
                    P.tt(actT[:, j, ts_], t2[:], gbc[:, ts_], ALU.mult, eng="pool")
        for dc in range(8):
            for tt in range(4):
                ts_ = slice(tt * 512, (tt + 1) * 512)
                ps = P.psum()
                for j in range(len(grp)):
                    P.mm(ps[:, :], wdb[:, j, dc * 128:(dc + 1) * 128], actT[:, j, ts_], start=(j == 0), stop=(j == len(grp) - 1))
                xk = (xacc.key, dc, tt)
                P.tt(xacc[:, dc, ts_], xacc[:, dc, ts_], ps[:, :], ALU.add, rk=[xk, ps[:, :]], wk=[xk])


def ffn_bufs(P):
    wgs = [P.alloc("wgs%d" % i, [128, 8, 128], BF16) for i in range(2)]
    wus = [P.alloc("wus%d" % i, [128, 8, 128], BF16) for i in range(2)]
    wds = [P.alloc("wds%d" % i, [128, 4, 1024], BF16) for i in range(2)]
    actTs = [P.alloc("actT%d" % i, [128, 4, T], BF16) for i in range(2)]
    t1s = [P.alloc("ft1_%d" % i, [128, 512], F32) for i in range(2)]
    t2s = [P.alloc("ft2_%d" % i, [128, 512], F32) for i in range(2)]
    return wgs, wus, wds, actTs, t1s, t2s


def load_xacc(P, xacc, x_src):
    for q4 in range(4):
        ks = [(xacc.key, dc, tt) for dc in (2 * q4, 2 * q4 + 1) for tt in range(4)]
        P.dma("sp", xacc[:, q4 * 2:q4 * 2 + 2, :], x_src[q4 * 256:(q4 + 1) * 256, :].rearrange("(c p) t -> p c t", p=128),
              rk=[x_src], wk=ks)


def store_xacc(P, xacc, x_dst):
    for q4 in range(4):
        ks = [(xacc.key, dc, tt) for dc in (2 * q4, 2 * q4 + 1) for tt in range(4)]
        P.dma("sp", x_dst[q4 * 256:(q4 + 1) * 256, :].rearrange("(c p) t -> p c t", p=128), xacc[:, q4 * 2:q4 * 2 + 2, :],
              rk=ks, wk=[x_dst])


def dense_ffn_phase(P, C, X, L, x_src, x_dst):
    inp = X.inp
    i = L // 2
    m0 = P.mark()
    hT = P.alloc("hTf", [128, 8, T], BF16)
    rmsnorm_T(P, C, x_src, inp["ffn_norm_g"][L], hT, T)
    xacc = P.alloc("xacc", [128, 8, T], F32)
    load_xacc(P, xacc, x_src)
    bufs = ffn_bufs(P)
    ffn_core(P, C, X, hT, xacc, inp["dense_wg"][i], inp["dense_wu"][i], inp["dense_wd"][i], 2816, bufs)
    store_xacc(P, xacc, x_dst)
    P.release(m0)


def moe_phase(P, C, X, L, x_src, x_dst):
    inp = X.inp
    i = L // 2
    m0 = P.mark()
    hT = P.alloc("hTm", [128, 8, T], BF16)
    gwT = P.alloc("gwT", [8, T], F32)
    oh = P.alloc("oh8", [8, 1024], F32)
    P.dma("sp", oh[:], X.cin["onehot8"])
    m1 = P.mark()
    lgT = P.alloc("lgT", [8, T], F32)
    rw = P.alloc("rw", [128, 8, 8], F32)
    P.dma("sp", rw[:], inp["router_w"][i].rearrange("(c p) e -> p c e", p=128))
    h32s = [P.alloc("h32_%d" % k, [128, 512], F32) for k in range(2)]
    st = {"ps": None, "n": 0}

    def cb(tt, dc, xt, gcol, r):
        h32 = h32s[st["n"] % 2]
        st["n"] += 1
        P.stt(h32[:], xt[:, dc, :], gcol[:, dc, :], r[:], ALU.mult, ALU.mult)
        P.copy(hT[:, dc, tt * 512:(tt + 1) * 512], h32[:], eng="act")
        if dc == 0:
            st["ps"] = P.psum()
        P.mm(st["ps"][0:8, :], rw[:, dc, :], h32[:], start=(dc == 0), stop=(dc == 7))
        if dc == 7:
            P.copy(lgT[0:8, tt * 512:(tt + 1) * 512], st["ps"][0:8, :], eng="act")

    rmsnorm_T(P, C, x_src, inp["ffn_norm_g"][L], hT, T, h32cb=cb)
    lg = P.alloc("lg", [128, 16, 8], F32)
    mx = P.alloc("mxr", [128, 16, 8], F32)
    gw = P.alloc("gw", [128, 16, 8], F32)
    tmp = P.alloc("rtmp", [128, 16, 8], F32)
    g1 = P.alloc("g1", [128, 16], F32)
    g2 = P.alloc("g2_", [128, 16], F32)
    ps = P.psum()
    for tile in range(16):
        P.mm(ps[:, tile * 8:(tile + 1) * 8], lgT[0:8, tile * 128:(tile + 1) * 128], C["ident_f"][0:8, 0:8])
    P.copy(lg[:].rearrange("p a b -> p (a b)"), ps[:, 0:128], eng="dve")
    for tile in range(16):
        P.op("dve", lambda e, tile=tile: e.max(out=mx[:, tile, :], in_=lg[:, tile, :]), reads=[lg], writes=[mx])
    P.tt(g1[:], mx[:, :, 0], mx[:, :, 1], ALU.subtract)
    P.act(g1[:], g1[:], AF.Sigmoid)
    P.ts(g2[:], g1[:], -1.0, 1.0, ALU.mult, ALU.add)
    P.tt(gw[:], lg[:], mx[:, :, 0:1].to_broadcast([128, 16, 8]), ALU.is_equal)
    P.tt(gw[:], gw[:], g1[:].unsqueeze(2).to_broadcast([128, 16, 8]), ALU.mult)
    P.tt(tmp[:], lg[:], mx[:, :, 1:2].to_broadcast([128, 16, 8]), ALU.is_equal)
    P.tt(tmp[:], tmp[:], g2[:].unsqueeze(2).to_broadcast([128, 16, 8]), ALU.mult)
    P.tt(gw[:], gw[:], tmp[:], ALU.add)
    for t4 in range(4):
        ps = P.psum()
        for j in range(4):
            tile = t4 * 4 + j
            P.mm(ps[0:8, j * 128:(j + 1) * 128], gw[:, tile, :], C["ident_f"][:, :])
        P.copy(gwT[0:8, t4 * 512:(t4 + 1) * 512], ps[0:8, :], eng="act")
    P.release(m1)
    gbc = P.alloc("gbc", [128, T], F32)
    xacc = P.alloc("xaccm", [128, 8, T], F32)
    load_xacc(P, xacc, x_src)
    bufs = ffn_bufs(P)
    for e in range(8):
        for tt in range(4):
            ps = P.psum()
            P.mm(ps[:, :], oh[0:8, e * 128:(e + 1) * 128], gwT[0:8, tt * 512:(tt + 1) * 512])
            P.copy(gbc[:, tt * 512:(tt + 1) * 512], ps[:, :], eng="act")
        ffn_core(P, C, X, hT, xacc, inp["exp_wg"][i][e], inp["exp_wu"][i][e], inp["exp_wd"][i][e], 3584, bufs, gbc=gbc)
    store_xacc(P, xacc, x_dst)
    P.release(m0)


def final_phase(P, C, X, x_src, out):
    stg = [P.alloc("fstg%d" % i, [128, 512], F32) for i in range(4)]
    st = {"n": 0}

    def cb(tt, dc, xt, gcol, r):
        s = stg[st["n"] % 4]
        st["n"] += 1
        P.stt(s[:], xt[:, dc, :], gcol[:, dc, :], r[:], ALU.mult, ALU.mult)
        P.dma("sp", out[dc * 128:(dc + 1) * 128, tt * 512:(tt + 1) * 512], s[:], wk=[("outT", dc, tt)])

    rmsnorm_T(P, C, x_src, X.inp["final_norm_g"], None, T, h32cb=cb)


PARAM_SPEC = {
    "w_in_first": [1024, 3096], "w_in_rest": [1, 1024, 3128], "shift_mu_first": [1792], "shift_mu_rest": [1, 1824],
    "mix_norm_g": [2, 1024], "rw_w0": [2, 512], "rw_w2": [2, 64, 512], "rw_a0": [2, 512], "rw_a2": [2, 64, 512],
    "rw_g2": [2, 128, 512], "rw_k_k": [2, 512], "rw_k_a": [2, 512], "rw_r_k": [2, 8, 64], "rw_ln_w": [2, 512],
    "rw_ln_b": [2, 512], "rw_v0": [1, 512], "rw_v2": [1, 32, 512], "cmp_k_pe": [2, 32, 64], "cmp_k_w1": [2, 2048, 128],
    "cmp_k_w2": [2, 128, 64], "cmp_v_pe": [2, 32, 64], "cmp_v_w1": [2, 2048, 128], "cmp_v_w2": [2, 128, 64],
    "w_out": [2, 1024, 1024], "cross_norm_g": [2, 1024], "mem_norm_g": [2, 1024], "cross_wq": [2, 1024, 1024],
    "cross_wkv": [2, 1024, 2048], "cross_wo": [2, 1024, 1024], "ffn_norm_g": [2, 1024], "dense_wg": [1, 1024, 2816],
    "dense_wu": [1, 1024, 2816], "dense_wd": [1, 2816, 1024], "router_w": [1, 1024, 8], "exp_wg": [1, 8, 1024, 3584],
    "exp_wu": [1, 8, 1024, 3584], "exp_wd": [1, 8, 3584, 1024], "final_norm_g": [1024],
}


def build_full(upto=99, dbg_names=()):
    P = Prog()
    dr = lambda name, shape, kind="ExternalInput", dt=F32: P.dram(name, shape, dt, kind).ap()
    X = Ctx()
    X.inp = {k: dr(k, v) for k, v in PARAM_SPEC.items()}
    xT = dr("xT", [D, T])
    X.memT = dr("memT", [D, 256])
    X.cin = {k: dr("c_" + k, list(v.shape)) for k, v in host_consts().items()}
    X.vfirst = dr("vfirst", [512, T], "Internal")
    X.ymix = dr("ymix", [1024, T], "Internal", BF16)
    xres = dr("xres", [D, T], "Internal")
    outT = dr("outT", [D, T], "ExternalOutput")
    X.dbg = {}
    C = load_consts(P, X.cin)
    x_cur = xT
    step = 0

    def go():
        nonlocal step
        step += 1
        return step <= upto

    for L in range(2):
        if not go():
            break
        m = P.mark()
        X.hT = P.alloc("hT", [128, 8, T], BF16)
        rmsnorm_T(P, C, x_cur, X.inp["mix_norm_g"][L], X.hT, T)
        if L == 0:
            X.w_in, X.mu, X.n_rw = X.inp["w_in_first"], X.inp["shift_mu_first"], 1792
        else:
            X.w_in, X.mu, X.n_rw = X.inp["w_in_rest"][0], X.inp["shift_mu_rest"][0], 1824
        rwkv_phase(P, C, X, L)
        nsa_phase(P, C, X, L)
        P.release(m)
        wout_phase(P, C, X, L, x_cur, xres)
        x_cur = xres
        if not go():
            break
        cross_phase(P, C, X, L, x_cur, xres)
        if not go():
            break
        if L % 2 == 0:
            dense_ffn_phase(P, C, X, L, x_cur, xres)
        else:
            moe_phase(P, C, X, L, x_cur, xres)
    if upto >= 99:
        final_phase(P, C, X, x_cur, outT)
    else:
        m = P.mark()
        xacc = P.alloc("dump", [128, 8, T], F32)
        load_xacc(P, xacc, x_cur)
        store_xacc(P, xacc, outT)
        P.release(m)
    print("ops:", {e: len(P.ops[e]) for e in P.ops})
    return P.finish()


def make_in_maps(inputs, cores):
    hc = host_consts()
    maps = []
    for b in cores:
        m = {k: np.ascontiguousarray(inputs[k], dtype=np.float32) for k in PARAM_SPEC}
        m["xT"] = np.ascontiguousarray(inputs["x"][b].T)
        m["memT"] = np.ascontiguousarray(inputs["mem"][b].T)
        for k, v in hc.items():
            m["c_" + k] = v
        maps.append(m)
    return maps


_NC = None


def kernel(**inputs):
    global _NC
    if _NC is None:
        _NC = build_full(99)
    maps = make_in_maps(inputs, list(range(8)))
    res = run_bass_kernel_spmd(_NC, maps, core_ids=list(range(8)))
    out = np.stack([np.ascontiguousarray(res.results[b]["outT"].T) for b in range(8)], 0)
    return out.astype(np.float32)
```
